# Optimizing a Trainium2 kernel written in Bass

```python
import math
import jax, jax.numpy as jnp
from jax import lax
import numpy as np

D_MODEL = 1024
BATCH = 16
SEQ = 2048
DEPTH = 1

ATT_HEADS = 8
ATT_KV_HEADS = 2
ATT_Q_PER_KV = ATT_HEADS // ATT_KV_HEADS
ATT_HEAD_DIM = 64
WINDOW = 128
ATT_BLOCK = 128
NEG_INF = -1e30
RET_HEADS = 8
RET_DK = 64
RET_DV = 128
RET_CHUNK = 128
ATT_Q_W = ATT_HEADS * ATT_HEAD_DIM
ATT_KV_W = ATT_KV_HEADS * ATT_HEAD_DIM
RET_QK_W = RET_HEADS * RET_DK
RET_V_W = RET_HEADS * RET_DV
SPLIT_SIZES = (ATT_Q_W, ATT_KV_W, ATT_KV_W, RET_QK_W, RET_QK_W, RET_V_W, RET_V_W, D_MODEL, D_MODEL)
SPLIT_POINTS = tuple(int(v) for v in np.cumsum(SPLIT_SIZES)[:-1])
IN_W = int(sum(SPLIT_SIZES))
N_GROUPS = 4
EXPERTS_PER_GROUP = 8
N_EXPERTS = N_GROUPS * EXPERTS_PER_GROUP
TOP_K = 2
D_EXPERT = 512
MOE_BLOCK = 128
EPS = 1e-6

kernel_name = "hybrid_swa_retention_hiermoe_block"


def rms_norm(x, g):
    xf = x.astype(jnp.float32)
    y = xf * lax.rsqrt(jnp.mean(xf * xf, axis=-1, keepdims=True) + EPS)
    return (y * g.astype(jnp.float32)).astype(x.dtype)


def alibi_slopes():
    return 2.0 ** (-8.0 * jnp.arange(1, ATT_HEADS + 1, dtype=jnp.float32) / ATT_HEADS)


def sliding_window_attention(q, k, v, sinks):
    B, S = q.shape[0], q.shape[1]
    N, C = S // ATT_BLOCK, ATT_BLOCK
    qb = q.reshape(B, N, C, ATT_KV_HEADS, ATT_Q_PER_KV, ATT_HEAD_DIM).astype(jnp.float32)
    kb = k.reshape(B, N, C, ATT_KV_HEADS, ATT_HEAD_DIM).astype(jnp.float32)
    vb = v.reshape(B, N, C, ATT_KV_HEADS, ATT_HEAD_DIM).astype(jnp.float32)

    def band(t):
        prev = jnp.pad(t, ((0, 0), (1, 0), (0, 0), (0, 0), (0, 0)))[:, :-1]
        return jnp.concatenate([prev, t], axis=2)

    kband, vband = band(kb), band(vb)
    s = jnp.einsum("bnikgd,bnjkd->bnkgij", qb, kband) * (ATT_HEAD_DIM ** -0.5)
    qi = jnp.arange(C)[:, None] + C
    kj = jnp.arange(2 * C)[None, :]
    dist = qi - kj
    in_window = (dist >= 0) & (dist < WINDOW)
    blk = jnp.arange(N)[:, None, None]
    valid = in_window[None] & ((blk > 0) | (kj[None] >= C))
    slopes = alibi_slopes().reshape(ATT_KV_HEADS, ATT_Q_PER_KV, 1, 1)
    s = s - slopes * dist.astype(jnp.float32)
    s = jnp.where(valid[None, :, None, None], s, NEG_INF)
    sink = sinks.astype(jnp.float32).reshape(ATT_KV_HEADS, ATT_Q_PER_KV, 1, 1)
    m = jnp.maximum(jnp.max(s, axis=-1, keepdims=True), sink)
    p = jnp.exp(s - m)
    denom = jnp.sum(p, axis=-1, keepdims=True) + jnp.exp(sink - m)
    o = jnp.einsum("bnkgij,bnjkd->bnikgd", p / denom, vband)
    return o.reshape(B, S, ATT_Q_W).astype(q.dtype)


def retention(q, k, v):
    B, S = q.shape[0], q.shape[1]
    N, C = S // RET_CHUNK, RET_CHUNK
    log_g = jnp.log(1.0 - 2.0 ** (-5.0 - jnp.arange(RET_HEADS, dtype=jnp.float32)))
    qc = q.reshape(B, N, C, RET_HEADS, RET_DK).astype(jnp.float32)
    kc = k.reshape(B, N, C, RET_HEADS, RET_DK).astype(jnp.float32) * (RET_DK ** -0.5)
    vc = v.reshape(B, N, C, RET_HEADS, RET_DV).astype(jnp.float32)
    pos = jnp.arange(C, dtype=jnp.float32)
    diff = pos[:, None] - pos[None, :]
    decay = jnp.where(diff >= 0, jnp.exp(jnp.maximum(diff, 0.0)[None] * log_g[:, None, None]), 0.0)
    scores = jnp.einsum("bnihd,bnjhd->bnhij", qc, kc) * decay
    inner = jnp.einsum("bnhij,bnjhe->bnihe", scores, vc)
    k_dec = kc * jnp.exp((C - 1 - pos)[:, None] * log_g)[None, None, :, :, None]
    kv = jnp.einsum("bnjhd,bnjhe->nbhde", k_dec, vc)
    chunk_decay = jnp.exp(C * log_g)[None, :, None, None]

    def step(state, kv_n):
        return state * chunk_decay + kv_n, state

    _, state_prev = lax.scan(step, jnp.zeros((B, RET_HEADS, RET_DK, RET_DV), jnp.float32), kv)
    q_dec = qc * jnp.exp((pos + 1.0)[:, None] * log_g)[None, None, :, :, None]
    cross = jnp.einsum("bnihd,nbhde->bnihe", q_dec, state_prev)
    y = (inner + cross).reshape(B, S, RET_HEADS, RET_DV)
    mu = jnp.mean(y, axis=-1, keepdims=True)
    var = jnp.mean(jnp.square(y - mu), axis=-1, keepdims=True)
    return (y - mu) * lax.rsqrt(var + EPS)


def gated_mixer(h, w_in, b_in, attn_sinks, w_attn_up, w_ret_up, w_out):
    B, S = h.shape[0], h.shape[1]
    proj = h @ w_in + b_in
    aq, ak, av, rq, rk, rv, rg, ga, gr = jnp.split(proj, SPLIT_POINTS, axis=-1)
    attn = sliding_window_attention(
        aq.reshape(B, S, ATT_HEADS, ATT_HEAD_DIM),
        ak.reshape(B, S, ATT_KV_HEADS, ATT_HEAD_DIM),
        av.reshape(B, S, ATT_KV_HEADS, ATT_HEAD_DIM), attn_sinks)
    attn = attn @ w_attn_up
    ret = retention(rq.reshape(B, S, RET_HEADS, RET_DK),
                    rk.reshape(B, S, RET_HEADS, RET_DK),
                    rv.reshape(B, S, RET_HEADS, RET_DV))
    ret = (ret.reshape(B, S, RET_V_W) * jax.nn.silu(rg.astype(jnp.float32))).astype(h.dtype)
    ret = ret @ w_ret_up
    mix = jax.nn.sigmoid(ga) * attn + jax.nn.sigmoid(gr) * ret
    return mix @ w_out


def hierarchical_moe(h, w_group_router, b_group_router, w_expert_router, b_expert_router,
                     w_gate_e, w_up_e, w_down_e):
    B, S, D = h.shape
    T = B * S
    A = T * TOP_K
    P = A + N_EXPERTS * MOE_BLOCK
    NB = P // MOE_BLOCK
    ht = h.reshape(T, D)
    g_logits = (ht @ w_group_router + b_group_router).astype(jnp.float32)
    g_probs = jax.nn.softmax(g_logits, axis=-1)
    g_idx = jnp.argmax(g_logits, axis=-1)
    g_prob = jnp.take_along_axis(g_probs, g_idx[:, None], axis=-1)
    e_logits = (ht @ w_expert_router + b_expert_router).astype(jnp.float32)
    e_logits = e_logits.reshape(T, N_GROUPS, EXPERTS_PER_GROUP)
    e_in_group = jnp.take_along_axis(e_logits, g_idx[:, None, None], axis=1)[:, 0]
    top_vals, top_idx = lax.top_k(e_in_group, TOP_K)
    weights = g_prob * jax.nn.softmax(top_vals, axis=-1)
    expert_ids = (g_idx[:, None] * EXPERTS_PER_GROUP + top_idx).reshape(A).astype(jnp.int32)
    tok_ids = jnp.arange(A, dtype=jnp.int32) // TOP_K
    order = jnp.argsort(expert_ids)
    sorted_e = expert_ids[order]
    counts = jnp.zeros((N_EXPERTS,), jnp.int32).at[expert_ids].add(1)
    pad_sizes = ((counts + MOE_BLOCK - 1) // MOE_BLOCK) * MOE_BLOCK
    pad_end = jnp.cumsum(pad_sizes)
    pad_start = pad_end - pad_sizes
    cnt_start = jnp.cumsum(counts) - counts
    dest_sorted = pad_start[sorted_e] + (jnp.arange(A, dtype=jnp.int32) - cnt_start[sorted_e])
    dest = jnp.zeros((A,), jnp.int32).at[order].set(dest_sorted)
    buf_tok = jnp.full((P,), T, jnp.int32).at[dest].set(tok_ids)
    h_pad = jnp.concatenate([ht, jnp.zeros((1, D), ht.dtype)], axis=0)
    x_blocks = h_pad[buf_tok].reshape(NB, MOE_BLOCK, D)
    block_start = jnp.arange(NB, dtype=jnp.int32) * MOE_BLOCK
    block_e = jnp.minimum(jnp.searchsorted(pad_end, block_start, side="right"), N_EXPERTS - 1)

    def expert_block(args):
        xb, e = args
        return (jax.nn.silu(xb @ w_gate_e[e]) * (xb @ w_up_e[e])) @ w_down_e[e]

    y_buf = lax.map(expert_block, (x_blocks, block_e)).reshape(P, D)
    y = jnp.sum(y_buf[dest].reshape(T, TOP_K, D) * weights[..., None].astype(h.dtype), axis=1)
    return y.reshape(B, S, D)


def setup_inputs(seed: int = 0) -> dict:
    key = jax.random.key(seed)
    ks = jax.random.split(key, 20)
    L, D = DEPTH, D_MODEL
    f32 = jnp.float32

    def nrm(k, shape, scale):
        return jax.random.normal(k, shape, f32) * scale

    return {
        "x": jax.random.normal(ks[0], (BATCH, SEQ, D), f32),
        "norm_mix_g": 1.0 + nrm(ks[1], (L, D), 0.02),
        "w_in": nrm(ks[2], (L, D, IN_W), D ** -0.5),
        "b_in": nrm(ks[3], (L, IN_W), 0.02),
        "attn_sinks": nrm(ks[4], (L, ATT_HEADS), 0.5),
        "w_attn_up": nrm(ks[5], (L, ATT_Q_W, D), ATT_Q_W ** -0.5),
        "w_ret_up": nrm(ks[6], (L, RET_V_W, D), RET_V_W ** -0.5),
        "w_out": nrm(ks[7], (L, D, D), D ** -0.5),
        "norm_ffn_g": 1.0 + nrm(ks[8], (L, D), 0.02),
        "w_group_router": nrm(ks[9], (L, D, N_GROUPS), D ** -0.5),
        "b_group_router": nrm(ks[10], (L, N_GROUPS), 0.01),
        "w_expert_router": nrm(ks[11], (L, D, N_EXPERTS), D ** -0.5),
        "b_expert_router": nrm(ks[12], (L, N_EXPERTS), 0.01),
        "w_gate_e": nrm(ks[13], (L, N_EXPERTS, D, D_EXPERT), D ** -0.5),
        "w_up_e": nrm(ks[14], (L, N_EXPERTS, D, D_EXPERT), D ** -0.5),
        "w_down_e": nrm(ks[15], (L, N_EXPERTS, D_EXPERT, D), D_EXPERT ** -0.5),
        "norm_final_g": 1.0 + nrm(ks[16], (D,), 0.02),
    }


def reference(x, norm_mix_g, w_in, b_in, attn_sinks, w_attn_up, w_ret_up, w_out,
              norm_ffn_g, w_group_router, b_group_router, w_expert_router, b_expert_router,
              w_gate_e, w_up_e, w_down_e, norm_final_g):
    for l in range(DEPTH):
        h = rms_norm(x, norm_mix_g[l])
        x = x + gated_mixer(h, w_in[l], b_in[l], attn_sinks[l], w_attn_up[l], w_ret_up[l], w_out[l])
        h = rms_norm(x, norm_ffn_g[l])
        x = x + hierarchical_moe(h, w_group_router[l], b_group_router[l], w_expert_router[l],
                                 b_expert_router[l], w_gate_e[l], w_up_e[l], w_down_e[l])
    return rms_norm(x, norm_final_g)
```

```python
import numpy as np
from contextlib import ExitStack
import concourse.bass as bass
import concourse.mybir as mybir
from concourse.bass_utils import run_bass_kernel_spmd

F32 = mybir.dt.float32
BF16 = mybir.dt.bfloat16
I32 = mybir.dt.int32
U32 = mybir.dt.uint32
AF = mybir.ActivationFunctionType
ALU = mybir.AluOpType
AX = mybir.AxisListType

D = 1024
SEQ = 2048
CH = 128
NBLK = SEQ // CH
IN_W = 5888
EPS = 1e-6
N_CORES = 8
NE = 32
DE = 512
CAP = 384
NSLOT = NE * CAP
OOB = NSLOT


class Sem:
    __slots__ = ("h", "name")

    def __init__(self, h, name):
        self.h = h
        self.name = name


class Eng:
    def __init__(self, kb, name, eng):
        self.kb = kb
        self.name = name
        self.eng = eng
        self.sem = None
        self.cnt = 0
        self.nsem = 0
        self.seen = {}

    def wait(self, sem, val):
        if self.seen.get(sem, 0) >= val:
            return
        self.eng.wait_ge(sem.h, val)
        self.seen[sem] = val

    def mark(self, instr):
        if self.sem is None or self.cnt >= 30000:
            self.sem = self.kb.newsem(f"{self.name}{self.nsem}")
            self.nsem += 1
            self.cnt = 0
        self.cnt += 1
        instr.then_inc(self.sem.h, 1)
        return (self.sem, self.cnt)


class DmaQ:
    def __init__(self, kb, E, n, name):
        self.E = E
        self.sems = [kb.newsem(f"{name}{i}") for i in range(n)]
        self.cnt = [0] * n
        self.i = 0

    def issue(self, fn):
        j = self.i % len(self.sems)
        self.i += 1
        sem = self.sems[j]
        if self.cnt[j] > 0:
            self.E.wait(sem, 16 * self.cnt[j])
        instr = fn()
        self.cnt[j] += 1
        instr.then_inc(sem.h, 16)
        return (sem, 16 * self.cnt[j])


class Buf:
    __slots__ = ("ws", "rs", "name")

    def __init__(self, name=""):
        self.ws = {}
        self.rs = {}
        self.name = name


class KB:
    def __init__(self, nc, es):
        self.nc = nc
        self.es = es
        self.cur = es
        self.pe = Eng(self, "pe", nc.tensor)
        self.act = Eng(self, "act", nc.scalar)
        self.dve = Eng(self, "dve", nc.vector)
        self.pool = Eng(self, "pool", nc.gpsimd)
        self.sp = Eng(self, "sp", nc.sync)
        self.qsp = DmaQ(self, self.sp, 20, "qsp")
        self.qpl = DmaQ(self, self.pool, 12, "qpl")
        self.qact = DmaQ(self, self.act, 6, "qact")
        self.banks = []
        self.bi = 0
        self.set_pools([0, 1], [2, 3], [4, 5, 6, 7])

    def newsem(self, name):
        return Sem(self.es.enter_context(self.nc.semaphore(name)), name)

    def sb(self, name, shape, dt):
        return self.cur.enter_context(self.nc.sbuf_tensor(name, shape, dt))

    def barrier(self):
        engs = (self.pe, self.act, self.dve, self.pool, self.sp)
        for E in engs:
            for E2 in engs:
                if E2 is not E and E2.sem is not None:
                    E.wait(E2.sem, E2.cnt)
            for qq in (self.qsp, self.qpl, self.qact) + ((self.qcast,) if hasattr(self, "qcast") else ()):
                for sem, c in zip(qq.sems, qq.cnt):
                    if c:
                        E.wait(sem, 16 * c)

    def op(self, E, fn, r=(), w=(), pw=(), q=None):
        need = {}

        def addall(d):
            for s, v in d.items():
                if need.get(s, 0) < v:
                    need[s] = v

        for b in r:
            addall(b.ws)
        for b in w:
            addall(b.ws)
            addall(b.rs)
        for b in pw:
            addall(b.rs)
        for s, v in need.items():
            E.wait(s, v)
        if q is None:
            ev = E.mark(fn())
        else:
            ev = q.issue(fn)
        s, v = ev
        for b in r:
            if b.rs.get(s, 0) < v:
                b.rs[s] = v
        for b in w:
            b.ws = {s: v}
            b.rs = {}
        for b in pw:
            if b.ws.get(s, 0) < v:
                b.ws[s] = v
        return ev

    def set_pools(self, A, B, Y=()):
        self.pools = {"A": list(A), "B": list(B), "Y": list(Y)}
        self.pctr = {"A": 0, "B": 0, "Y": 0}

    def bank(self, pool=None):
        if pool is None:
            b = self.banks[self.bi % len(self.banks)]
            self.bi += 1
            return b
        lst = self.pools[pool]
        b = self.banks[lst[self.pctr[pool] % len(lst)]]
        self.pctr[pool] += 1
        return b


def host_consts():
    j = np.arange(128)[:, None].astype(np.float64)
    i = np.arange(128)[None, :].astype(np.float64)
    slopes = 2.0 ** (-8.0 * np.arange(1, 9) / 8.0)
    mt = np.zeros((128, 2, 8, 128), np.float64)
    for h in range(8):
        dc = i - j
        mt[:, 1, h, :] = np.where(dc >= 0, np.exp(-slopes[h] * np.maximum(dc, 0)), 0.0)
        dp = i + 128 - j
        mt[:, 0, h, :] = np.where(dp < 128, np.exp(-slopes[h] * dp), 0.0)
    caus = (j <= i).astype(np.float64)
    gam = 1.0 - 2.0 ** (-5.0 - np.arange(8))
    qd = np.zeros((128, 4, 128))
    ks = np.zeros((128, 4, 128))
    gc = np.zeros((128, 4, 128))
    pos = np.arange(128)
    for c in range(4):
        for p in range(2):
            g = gam[2 * c + p]
            qd[64 * p:64 * p + 64, c, :] = (g ** (pos + 1.0))[None, :]
            ks[64 * p:64 * p + 64, c, :] = (g ** (-(pos + 1.0)) / 8.0)[None, :]
            gc[64 * p:64 * p + 64, c, :] = g ** 128.0
    ident = np.eye(128)
    tri = (j < i).astype(np.float64)
    ebase = np.tile((np.arange(NE) * CAP)[None, :], (128, 1))
    c = {
        "c_mtab": mt.reshape(128, -1), "c_caus": caus, "c_qd": qd.reshape(128, -1),
        "c_ks": ks.reshape(128, -1), "c_gc": gc.reshape(128, -1), "c_ident": ident,
        "c_tri": tri, "c_ebase": ebase,
    }
    return {k: np.ascontiguousarray(v, dtype=np.float32) for k, v in c.items()}


def build_nc(nseq=2, taps=(), stop_after=None):
    T = nseq * SEQ
    NT = T // 128
    nc = bass.Bass("TRN2", target_bir_lowering=False)

    def din(name, shape, dt=F32):
        return nc.dram_tensor(name, list(shape), dt, kind="ExternalInput").ap()

    def dint(name, shape, dt):
        return nc.dram_tensor(name, list(shape), dt, kind="Internal").ap()

    x = din("x", [T, D])
    norm_mix_g = din("norm_mix_g", [1, D])
    w_in = din("w_in", [D, IN_W])
    b_in = din("b_in", [1, IN_W])
    sinks = din("attn_sinks", [1, 8])
    w_au = din("w_attn_up", [512, D])
    w_ru = din("w_ret_up", [D, D])
    w_out = din("w_out", [D, D])
    norm_ffn_g = din("norm_ffn_g", [1, D])
    w_gr = din("w_group_router", [D, 4])
    b_gr = din("b_group_router", [1, 4])
    w_er = din("w_expert_router", [D, NE])
    b_er = din("b_expert_router", [1, NE])
    w_ge = din("w_gate_e", [NE, D, DE])
    w_ue = din("w_up_e", [NE, D, DE])
    w_de = din("w_down_e", [NE, DE, D])
    norm_fin_g = din("norm_final_g", [1, D])
    hc = host_consts()
    cin = {k: din(k, v.shape) for k, v in hc.items()}
    out = nc.dram_tensor("out", [T, D], F32, kind="ExternalOutput").ap()
    tapo = {}

    Wb = dint("Wb", [D, IN_W], BF16)
    Waub = dint("Waub", [128, 8, 512], BF16)
    Wrub = dint("Wrub", [D, D], BF16)
    Woutb = dint("Woutb", [D, D], BF16)
    X2 = dint("X2", [T, D], F32)
    Xg = dint("Xg", [NSLOT, D], BF16)
    Yg = dint("Yg", [NSLOT + 128, D], BF16)

    es = ExitStack()
    with es:
        kb = KB(nc, es)
        op = kb.op
        pe, act, dve, pool, sp = kb.pe, kb.act, kb.dve, kb.pool, kb.sp
        qsp, qpl = kb.qsp, kb.qpl
        V, S, G, P_ = nc.vector, nc.scalar, nc.gpsimd, nc.tensor
        sb = kb.sb
        for i in range(8):
            kb.banks.append((es.enter_context(nc.psum_tensor(f"bank{i}", [128, 512], F32)), Buf(f"bank{i}")))

        def tap(name, src_ap, shape, rbufs, dt=F32):
            if name not in taps:
                return
            if name not in tapo:
                tapo[name] = nc.dram_tensor("tap_" + name, list(shape), dt, kind="ExternalOutput").ap()
            return tapo[name]

        bWau, bWru, bWout = Buf(), Buf(), Buf()
        SRC = {"aq": 0, "ak": 512, "av": 640, "rq": 768, "rk": 1280, "rv": 1792, "rg": 2816, "ga": 3840, "gr": 4864}
        DST = {"rq": 0, "rk": 512, "rv": 1024, "rg": 2048, "aq": 3072, "ak": 3584, "av": 3712, "ga": 3840, "gr": 4864}
        WID = {"aq": 512, "ak": 128, "av": 128, "rq": 512, "rk": 512, "rv": 1024, "rg": 1024, "ga": 1024, "gr": 1024}
        WCH = [(0, 512), (512, 512), (1024, 512), (1536, 512), (2048, 512), (2560, 512), (3072, 512), (3584, 256),
               (3840, 512), (4352, 512), (4864, 512), (5376, 512)]
        BWc = [Buf(f"Wb{i}") for i in range(len(WCH))]

        def chunk_of(col):
            for i, (c0, n) in enumerate(WCH):
                if c0 <= col < c0 + n:
                    return i


        def ctile(name, key, shape, dt=F32, q=None):
            t = sb(name, shape, dt)
            b = Buf(name)
            if dt == F32:
                op(sp, lambda: nc.sync.dma_start(out=t[:], in_=cin[key]), w=[b], q=qsp)
            else:
                op(pool, lambda: G.dma_start(out=t[:], in_=cin[key]), w=[b], q=qpl)
            return t, b

        mtab, bmtab = ctile("mtab", "c_mtab", [128, 2 * 8 * 128])
        caus, bcaus = ctile("caus", "c_caus", [128, 128])
        qdtab, bqd = ctile("qdtab", "c_qd", [128, 512])
        kstab, bks = ctile("kstab", "c_ks", [128, 512])
        gctab, bgc = ctile("gctab", "c_gc", [128, 512])
        identb, bidb = ctile("identb", "c_ident", [128, 128], BF16)
        identf, bidf = ctile("identf", "c_ident", [128, 128])
        mtab4 = mtab[:].rearrange("p (a h i) -> p a h i", a=2, h=8, i=128)

        bconst = Buf("const")
        gmix = sb("gmix", [128, D], F32)
        op(sp, lambda: nc.sync.dma_start(out=gmix[:], in_=norm_mix_g.partition_broadcast(128)), w=[bconst], q=qsp)
        gffn = sb("gffn", [128, D], F32)
        bgffn = Buf()
        op(sp, lambda: nc.sync.dma_start(out=gffn[:], in_=norm_ffn_g.partition_broadcast(128)), w=[bgffn], q=qsp)
        bcol = sb("bcol", [128, 29], F32)
        bbcol = Buf("bcol")
        with nc.allow_non_contiguous_dma(reason="tiny bias column loads"):
            for c in range(4):
                op(sp, lambda c=c: nc.sync.dma_start(out=bcol[0:64, 8 + c:9 + c], in_=b_in[0:1, 64 * c:64 * c + 64].rearrange("o e -> e o")), pw=[bbcol], q=qsp)
                op(sp, lambda c=c: nc.sync.dma_start(out=bcol[64:128, 8 + c:9 + c], in_=b_in[0:1, 256 + 64 * c:256 + 64 * c + 64].rearrange("o e -> e o")), pw=[bbcol], q=qsp)
            for nm, m in (("rq", 0), ("rk", 4), ("ak", 12), ("ga", 13), ("gr", 21)):
                nchunk = WID[nm] // 128
                op(sp, lambda nm=nm, m=m, nchunk=nchunk: nc.sync.dma_start(
                    out=bcol[:, m:m + nchunk],
                    in_=b_in[0:1, SRC[nm]:SRC[nm] + WID[nm]].rearrange("o (m p) -> p (o m)", p=128)), pw=[bbcol], q=qsp)
        bhalf = sb("bhalf", [128, 16], F32)
        bbhalf = Buf()
        op(dve, lambda: V.tensor_scalar(out=bhalf[:], in0=bcol[:, 13:29], scalar1=0.5, scalar2=None, op0=ALU.mult), r=[bbcol], w=[bbhalf])
        brow_b = sb("brow_b", [1, 2176], BF16)
        bbrow = Buf()
        o = 0
        for nm in ("av", "rv", "rg"):
            op(pool, lambda o=o, nm=nm: G.dma_start(out=brow_b[0:1, o:o + WID[nm]], in_=b_in[0:1, SRC[nm]:SRC[nm] + WID[nm]]), pw=[bbrow], q=qpl)
            o += WID[nm]
        ones2 = sb("ones2", [1, 128], BF16)
        onesb = sb("onesb", [128, 128], BF16)
        mhalf = sb("mhalf", [128, 8], F32)
        bones = Buf()
        op(dve, lambda: V.memset(ones2[:], 1.0), w=[bones])
        op(dve, lambda: V.memset(onesb[:], 1.0), w=[bones])
        op(dve, lambda: V.memset(mhalf[:], -0.5), w=[bones])
        sk = sb("sk", [128, 2, 2], F32)
        esink = sb("esink", [128, 2, 2, 128], F32)
        bsk = Buf()
        for hf_ in range(2):
            for g_ in range(2):
                o_ = 4 * g_ + 2 * hf_
                op(sp, lambda hf_=hf_, g_=g_, o_=o_: nc.sync.dma_start(out=sk[64 * hf_:64 * hf_ + 64, g_, :], in_=sinks[0:1, o_:o_ + 2].partition_broadcast(64)),
                   pw=[bsk], q=qsp)
        op(act, lambda: S.activation(out=sk[:], in_=sk[:], func=AF.Exp), w=[bsk])
        op(dve, lambda: V.tensor_copy(out=esink[:].rearrange("p g c i -> p (g c) i"), in_=sk[:].rearrange("p g c -> p (g c)").unsqueeze(2).to_broadcast([128, 4, 128])),
           r=[bsk], w=[bconst])

        gfin = sb("gfin", [128, D], F32)
        bgfin = Buf()
        op(sp, lambda: nc.sync.dma_start(out=gfin[:], in_=norm_fin_g.partition_broadcast(128)), w=[bgfin], q=qsp)
        wr = sb("wr", [128, 8, 36], F32)
        bwr = Buf()
        with nc.allow_non_contiguous_dma(reason="small router weights"):
            op(sp, lambda: nc.sync.dma_start(out=wr[:, :, 0:4], in_=w_gr.rearrange("(k p) g -> p k g", p=128)), pw=[bwr], q=qsp)
            op(sp, lambda: nc.sync.dma_start(out=wr[:, :, 4:36], in_=w_er.rearrange("(k p) g -> p k g", p=128)), pw=[bwr], q=qsp)
        brt = sb("brt", [1, 36], F32)
        op(sp, lambda: nc.sync.dma_start(out=brt[:, 0:4], in_=b_gr), pw=[bwr], q=qsp)
        op(sp, lambda: nc.sync.dma_start(out=brt[:, 4:36], in_=b_er), pw=[bwr], q=qsp)
        ones_f = sb("ones_f", [1, 128], F32)
        op(dve, lambda: V.memset(ones_f[:], 1.0), pw=[bwr])
        trib, btrib = ctile("trib", "c_tri", [128, 128], BF16)
        ebase, bebase = ctile("ebase", "c_ebase", [128, NE])
        wts = sb("wts", [128, NT, 2], F32)
        bwts = Buf()
        dest_all = sb("dest_all", [128, NT, 2], I32)
        bdest = Buf()
        base_bc = sb("base_bc", [128, NE], F32)
        bbase = Buf()
        op(dve, lambda: V.memset(base_bc[:], 0.0), w=[bbase])
        bXg = Buf("Xg")
        bXg = Buf("Xg")
        bX2 = Buf("X2")
        bYg = Buf("Yg")
        breg = G.to_reg(NSLOT - 1)
        breg2 = G.to_reg(NSLOT)
        zt = sb("zt", [128, 1, D], BF16)
        bzt = Buf()
        op(dve, lambda: V.memset(zt[:], 0.0), w=[bzt])
        op(act, lambda: S.dma_start(out=Yg[NSLOT:NSLOT + 128, :], in_=zt[:, 0, :]), r=[bzt], pw=[bYg], q=kb.qact)
        bXz = Buf("Xg_zero")

        def zero_gen():
            for z_ in range(NSLOT // 128):
                op(sp, lambda z_=z_: nc.sync.dma_start(out=Xg[z_ * 128:(z_ + 1) * 128, :], in_=zt[:, 0, :]), r=[bzt], pw=[bXz], q=qsp)
                if z_ % 2 == 1:
                    yield

        zerog = zero_gen()

        def zero_tick():
            try:
                next(zerog)
            except StopIteration:
                pass

        def ensure_zero():
            for _ in zerog:
                pass
        bX2 = Buf("X2")
        bYg = Buf("Yg")

        qcast = DmaQ(kb, pool, 4, "qcast")
        kb.qcast = qcast
        NWC = 3
        wc = [kb.es.enter_context(nc.sbuf_tensor(f"wc{i}", [128, 8, 512], BF16)) for i in range(NWC)]
        bwc = [Buf() for _ in range(NWC)]
        wci = [0]
        Wb_v = Wb.rearrange("(k p) c -> p k c", p=128)
        Wru_v = Wrub.rearrange("(k p) c -> p k c", p=128)
        Wout_v = Woutb.rearrange("(k p) c -> p k c", p=128)
        win_v = w_in.rearrange("(k p) c -> p k c", p=128)
        wru_v = w_ru.rearrange("(k p) c -> p k c", p=128)
        wout_v = w_out.rearrange("(k p) c -> p k c", p=128)
        SECT = [("rq", 0), ("rk", 512), ("rv", 1024), ("rg", 2048), ("aq", 3072), ("ak", 3584), ("av", 3712), ("ga", 3840), ("gr", 4864)]

        def src_col(dcol):
            for nm, d0 in SECT:
                if d0 <= dcol < d0 + WID[nm]:
                    return SRC[nm] + (dcol - d0)

        def stage_chunk(pieces, store_dst, store_buf, n):
            i = wci[0] % NWC
            wci[0] += 1
            first = True
            for dst_fn, src in pieces:
                if first:
                    op(pool, lambda dst_fn=dst_fn, src=src: G.dma_start(out=dst_fn(wc[i]), in_=src), w=[bwc[i]], q=qcast)
                    first = False
                else:
                    op(pool, lambda dst_fn=dst_fn, src=src: G.dma_start(out=dst_fn(wc[i]), in_=src), pw=[bwc[i]], q=qcast)
            op(sp, lambda: nc.sync.dma_start(out=store_dst, in_=wc[i][:, :, 0:n]), r=[bwc[i]], pw=[store_buf], q=qsp)

        staged = set()

        def stage_gen():
            for ci in (6, 7, 0, 1, 2, 3, 4, 5, 8, 9, 10, 11):
                c0, n = WCH[ci]
                if ci == 6:
                    pcs = [((lambda t, c=c, h=h: t[:, :, 128 * c + 64 * h:128 * c + 64 * h + 64]), win_v[:, :, 256 * h + 64 * c:256 * h + 64 * c + 64])
                           for c in range(4) for h in range(2)]
                elif ci == 7:
                    pcs = [((lambda t: t[:, :, 0:128]), win_v[:, :, SRC["ak"]:SRC["ak"] + 128]),
                           ((lambda t: t[:, :, 128:256]), win_v[:, :, SRC["av"]:SRC["av"] + 128])]
                else:
                    sc_ = src_col(c0)
                    pcs = [((lambda t, n=n: t[:, :, 0:n]), win_v[:, :, sc_:sc_ + n])]
                with nc.allow_non_contiguous_dma(reason="one-time weight staging"):
                    stage_chunk(pcs, Wb_v[:, :, c0:c0 + n], BWc[ci], n)
                staged.add(ci)
                yield
            pcs = []
            for g_ in range(2):
                for hf_ in range(2):
                    for cc_ in range(2):
                        for j_ in range(2):
                            r0 = ((2 * g_ + hf_) * 2 + cc_) * 64
                            pcs.append(((lambda t, hf_=hf_, g_=g_, cc_=cc_, j_=j_: t[64 * hf_:64 * hf_ + 64, 4 * j_ + 2 * g_ + cc_, :]),
                                        w_au[r0:r0 + 64, 512 * j_:512 * j_ + 512]))
            stage_chunk(pcs, Waub, bWau, 512)
            staged.add("wau")
            yield
            for j in range(2):
                stage_chunk([((lambda t: t[:, :, 0:512]), wru_v[:, :, 512 * j:512 * j + 512])], Wru_v[:, :, 512 * j:512 * j + 512], bWru, 512)
                yield
            staged.add("wru")
            for j in range(2):
                stage_chunk([((lambda t: t[:, :, 0:512]), wout_v[:, :, 512 * j:512 * j + 512])], Wout_v[:, :, 512 * j:512 * j + 512], bWout, 512)
                yield
            staged.add("wout")

        stageg = stage_gen()
        stage_ctr = [0]

        def ensure_staged(key):
            while key not in staged:
                next(stageg)

        def stage_tick():
            stage_ctr[0] += 1
            if stage_ctr[0] % 3 == 0:
                try:
                    next(stageg)
                except StopIteration:
                    pass

        if "eager" in taps:
            for _ in stageg:
                pass
            kb.barrier()
        else:
            ensure_staged(7)
        es_mix = ExitStack()
        kb.cur = es_mix
        x_sb = sb("x_sb", [128, 2, D], F32)
        bx = [Buf(f"x{t}") for t in range(2)]
        ssq = sb("ssq", [128, 8], F32)
        rstd = sb("rstd", [128, 8], F32)
        bst = [Buf() for _ in range(4)]
        xn = [sb(f"xn{i}", [128, D], BF16) for i in range(2)]
        bxn = [Buf(), Buf()]
        xnT = sb("xnT", [128, 8, 512], BF16)
        bxnT = [Buf() for _ in range(4)]
        QT = sb("QT", [128, 4, 512], BF16)
        bQT = Buf()
        KT = sb("KT", [128, SEQ], BF16)
        bKT = [Buf() for _ in range(NBLK)]
        VA = sb("VA", [128, NBLK, 128], BF16)
        bVA = [Buf() for _ in range(NBLK)]
        qdT = sb("qdT", [128, 4, 512], BF16)
        bqdT = Buf()
        ksT = sb("ksT", [128, 4, 512], BF16)
        bksT = Buf()
        ta = sb("ta", [128, 8, 512], BF16)
        tr_ = sb("tr", [128, 8, 512], BF16)
        bta = [Buf() for _ in range(8)]
        btr = [Buf() for _ in range(8)]
        Vr = [sb(f"Vr{t}", [128, D], BF16) for t in range(4)]
        bVr = [Buf() for _ in range(4)]
        sg = [sb(f"sg{t}", [128, D], BF16) for t in range(4)]
        bsg = [Buf() for _ in range(4)]
        tg = sb("tg", [128, 512], F32)
        btg = Buf()
        eS = [sb(f"eS{i}", [128, 512], F32) for i in range(2)]
        beS = [Buf(), Buf()]
        PT = [sb(f"PT{i}", [128, 512], BF16) for i in range(4)]
        bPT = [Buf() for _ in range(4)]
        rden = sb("rden", [128, 256], F32)
        brden = Buf()
        attnT = sb("attnT", [128, 2, 2, 512], BF16)
        battn = [Buf() for _ in range(4)]
        scT = sb("scT", [128, 2, 4, 128], BF16)
        bscT = Buf()
        kstok = sb("kstok", [128, 4, 128], BF16)
        bkstok = Buf()
        state = sb("state", [128, 512], F32)
        stmp = sb("stmp", [128, 512], F32)
        state_bf = [sb(f"state_bf{i}", [128, 512], BF16) for i in range(2)]
        bstate = Buf()
        bstmp = Buf()
        bstbf = [Buf(), Buf()]
        st8_2 = [sb(f"st8_{i}", [128, 8, 8], F32) for i in range(2)]
        bst8_2 = [Buf(), Buf()]
        x2 = [sb(f"x2_{i}", [128, D], F32) for i in range(2)]
        bx2 = [Buf(), Buf()]
        ysq, bysq = x2[0], bx2[0]
        yn, byn = x2[1], bx2[1]
        retg = sb("retg", [128, D], BF16)
        bretg = Buf()
        junk, bjunk = retg, bretg
        retgT = sb("retgT", [128, 8, 512], BF16)
        bretgT = [Buf() for _ in range(4)]
        mixT = sb("mixT", [128, 8, 512], BF16)
        bmix = [Buf() for _ in range(8)]
        m1, m2, bm1, bm2 = eS[0], eS[1], beS[0], beS[1]
        h2f = sb("h2f", [128, D], F32)
        bh2f = Buf()
        h2b = [sb(f"h2b{i}", [128, D], BF16) for i in range(2)]
        bh2b = [Buf(), Buf()]
        h2T = sb("h2T", [128, 8, 128], F32)
        bh2T = Buf()
        R = sb("R", [128, 384], F32)
        bR = Buf()
        selb = sb("selb", [128, NE], BF16)
        bselb = Buf()
        rs = sb("rs", [128, 2], F32)
        brs = Buf()

        if "mem" in taps:
            print("SBUF bytes remaining/partition after mixer alloc:", nc.sbuf_bytes_remaining)
        def bview(bank_t, a, b_):
            return bank_t[:].rearrange("p (a b) -> p a b", a=a, b=b_)

        def load_w(src_ap, rb, n=512):
            i = wci[0] % NWC
            wci[0] += 1
            op(sp, lambda: nc.sync.dma_start(out=wc[i][:, :, 0:n], in_=src_ap), r=[rb], w=[bwc[i]], q=qsp)
            return wc[i], bwc[i]


        def route_a(tt, xi):
            lg, gmax, ngmax, gsum, gprob = R[:, 0:36], R[:, 36:37], R[:, 37:38], R[:, 38:39], R[:, 39:40]
            goh, t1, jk4 = R[:, 40:44], R[:, 44:48], R[:, 48:52]
            em, top8, sel1, sel2 = R[:, 64:96], R[:, 96:104], R[:, 104:136], R[:, 136:168]
            dlt, e2, den, rr = R[:, 168:169], R[:, 169:170], R[:, 170:171], R[:, 171:172]
            posf, okm, nbig, tmp, tmp1, d12 = R[:, 176:208], R[:, 208:240], R[:, 240:272], R[:, 272:304], R[:, 304:336], R[:, 336:338]
            hb = tt % 2
            op(act, lambda: S.activation(out=junk[:], in_=x2[xi][:], func=AF.Square, accum_out=rs[:, 0:1]), r=[bx2[xi]], w=[bjunk, brs])
            op(dve, lambda: V.tensor_scalar(out=rs[:, 0:1], in0=rs[:, 0:1], scalar1=1.0 / D, scalar2=EPS, op0=ALU.mult, op1=ALU.add), w=[brs])
            op(pool, lambda: G.tensor_tensor(out=rs[:, 1:2], in0=rs[:, 0:1], in1=mhalf[:, 0:1], op=ALU.pow), r=[bones], w=[brs])
            op(dve, lambda: V.scalar_tensor_tensor(out=h2f[:], in0=x2[xi][:], scalar=rs[:, 1:2], in1=gffn[:], op0=ALU.mult, op1=ALU.mult),
               r=[bx2[xi], brs, bgffn], w=[bh2f])
            op(pool, lambda: G.tensor_copy(out=h2b[hb][:], in_=h2f[:]), r=[bh2f], w=[bh2b[hb]])

        def route_b(tt, xi):
            lg, gmax, ngmax, gsum, gprob = R[:, 0:36], R[:, 36:37], R[:, 37:38], R[:, 38:39], R[:, 39:40]
            goh, t1, jk4 = R[:, 40:44], R[:, 44:48], R[:, 48:52]
            em, top8, sel1, sel2 = R[:, 64:96], R[:, 96:104], R[:, 104:136], R[:, 136:168]
            dlt, e2, den, rr = R[:, 168:169], R[:, 169:170], R[:, 170:171], R[:, 171:172]
            posf, okm, nbig, tmp, tmp1, d12 = R[:, 176:208], R[:, 208:240], R[:, 240:272], R[:, 272:304], R[:, 304:336], R[:, 336:338]
            hb = tt % 2
            for j in range(2):
                bt, bb = kb.bank("B")

                def trf(bt=bt, j=j):
                    ins = None
                    for k in range(4):
                        kk = 4 * j + k
                        ins = P_.transpose(out=bt[:, k * 128:(k + 1) * 128], in_=h2f[:, kk * 128:(kk + 1) * 128], identity=identf[:])
                    return ins
                op(pe, trf, r=[bh2f, bidf], w=[bb])
                op(act, lambda bt=bt, j=j: S.copy(out=h2T[:, 4 * j:4 * j + 4, :], in_=bview(bt, 4, 128)), r=[bb], pw=[bh2T])
            yield
            bt, bb = kb.bank("B")

            def mmr_(bt=bt):
                P_.matmul(bt[:, 0:36], lhsT=ones_f[:], rhs=brt[:], start=True, stop=False)
                ins = None
                for k in range(8):
                    ins = P_.matmul(bt[:, 0:36], lhsT=h2T[:, k, :], rhs=wr[:, k, :], start=False, stop=(k == 7))
                return ins
            op(pe, mmr_, r=[bh2T, bwr], w=[bb])
            op(dve, lambda: V.tensor_copy(out=lg, in_=bt[:, 0:36]), r=[bb], w=[bR])

            def c(fn, E=dve, **kw):
                op(E, fn, w=[bR], **kw)
            c(lambda: V.tensor_reduce(out=gmax, in_=R[:, 0:4], axis=AX.X, op=ALU.max))
            c(lambda: V.tensor_scalar(out=goh, in0=R[:, 0:4], scalar1=gmax, scalar2=None, op0=ALU.is_equal))
            c(lambda: V.tensor_scalar(out=t1, in0=goh, scalar1=-1.0, scalar2=1e30, op0=ALU.add, op1=ALU.mult))
            c(lambda: V.tensor_tensor(out=em.rearrange("p (g e) -> p g e", g=4), in0=R[:, 4:36].rearrange("p (g e) -> p g e", g=4),
                                      in1=goh.unsqueeze(2).to_broadcast([128, 4, 8]), op=ALU.mult))
            c(lambda: V.tensor_tensor(out=em.rearrange("p (g e) -> p g e", g=4), in0=em.rearrange("p (g e) -> p g e", g=4),
                                      in1=t1.unsqueeze(2).to_broadcast([128, 4, 8]), op=ALU.add))
            yield
            c(lambda: V.max(out=top8, in_=em))
            c(lambda: V.tensor_scalar(out=sel1, in0=em, scalar1=top8[:, 0:1], scalar2=None, op0=ALU.is_equal))
            c(lambda: V.tensor_scalar(out=sel2, in0=em, scalar1=top8[:, 1:2], scalar2=None, op0=ALU.is_equal))
            op(dve, lambda: V.tensor_tensor(out=selb[:], in0=sel1, in1=sel2, op=ALU.add), r=[bR], w=[bselb])
            yield
            btp, bbp = kb.bank("B")

            def mmp(btp=btp):
                P_.matmul(btp[:, 0:32], lhsT=trib[:], rhs=selb[:], start=True, stop=True)
                return P_.matmul(btp[:, 32:64], lhsT=onesb[:], rhs=selb[:], start=True, stop=True)
            op(pe, mmp, r=[bselb, btrib, bones], w=[bbp])
            c(lambda: V.tensor_scalar(out=ngmax, in0=gmax, scalar1=-1.0, scalar2=None, op0=ALU.mult))
            c(lambda: S.activation(out=jk4, in_=R[:, 0:4], func=AF.Exp, bias=ngmax, scale=1.0, accum_out=gsum), E=act)
            c(lambda: V.tensor_tensor(out=dlt, in0=top8[:, 1:2], in1=top8[:, 0:1], op=ALU.subtract))
            c(lambda: S.activation(out=e2, in_=dlt, func=AF.Exp), E=act)
            c(lambda: V.reciprocal(out=gprob, in_=gsum))
            c(lambda: V.tensor_scalar(out=den, in0=e2, scalar1=1.0, scalar2=None, op0=ALU.add))
            c(lambda: V.reciprocal(out=rr, in_=den))
            op(dve, lambda: V.scalar_tensor_tensor(out=wts[:, tt, 0:1], in0=gprob, scalar=0.5, in1=rr, op0=ALU.mult, op1=ALU.mult), r=[bR], pw=[bwts])
            op(dve, lambda: V.tensor_tensor(out=wts[:, tt, 1:2], in0=wts[:, tt, 0:1], in1=e2, op=ALU.mult), r=[bR], w=[bwts])
            yield
            op(dve, lambda: V.tensor_tensor(out=posf, in0=btp[:, 0:32], in1=base_bc[:], op=ALU.add), r=[bbp, bbase], w=[bR])
            op(dve, lambda: V.tensor_tensor(out=base_bc[:], in0=btp[:, 32:64], in1=base_bc[:], op=ALU.add), r=[bbp], w=[bbase])
            yield
            c(lambda: V.tensor_scalar(out=okm, in0=posf, scalar1=float(CAP), scalar2=None, op0=ALU.is_lt))
            c(lambda: V.tensor_scalar(out=nbig, in0=okm, scalar1=-1.0, scalar2=-float(OOB), op0=ALU.add, op1=ALU.mult))
            c(lambda: V.tensor_tensor(out=tmp, in0=posf, in1=ebase[:], op=ALU.add), r=[bebase])
            c(lambda: V.tensor_tensor(out=tmp, in0=tmp, in1=okm, op=ALU.mult))
            c(lambda: V.tensor_tensor(out=tmp, in0=tmp, in1=nbig, op=ALU.add))
            c(lambda: V.tensor_tensor(out=tmp1, in0=tmp, in1=sel1, op=ALU.mult))
            c(lambda: V.tensor_reduce(out=d12[:, 0:1], in_=tmp1, axis=AX.X, op=ALU.add))
            c(lambda: V.tensor_tensor(out=tmp1, in0=tmp, in1=sel2, op=ALU.mult))
            c(lambda: V.tensor_reduce(out=d12[:, 1:2], in_=tmp1, axis=AX.X, op=ALU.add))
            op(dve, lambda: V.tensor_copy(out=dest_all[:, tt, :], in_=d12), r=[bR], pw=[bdest])
            for k in range(2):
                op(pool, lambda k=k: G.indirect_dma_start(out=Xg[:, :], out_offset=bass.IndirectOffsetOnAxis(ap=dest_all[:, tt, k:k + 1], axis=0),
                                                          in_=h2b[hb][:], in_offset=None, bounds_check=breg, oob_is_err=False),
                   r=[bh2b[hb], bdest, bXz], pw=[bXg], q=qpl)
            if "route" in taps and tt == 0:
                tapo["R"] = nc.dram_tensor("tap_R", [128, 384], F32, kind="ExternalOutput").ap()
                op(sp, lambda: nc.sync.dma_start(out=tapo["R"], in_=R[:]), r=[bR], q=qsp)

        def gen_norm(s, q):
            tok0 = s * SEQ + q * 512
            for t in range(4):
                xb_ = t % 2
                op(sp, lambda t=t, xb_=xb_: nc.sync.dma_start(out=x_sb[:, xb_, :], in_=x[tok0 + t * 128: tok0 + (t + 1) * 128, :]), w=[bx[xb_]], q=qsp)
                op(act, lambda t=t, xb_=xb_: S.activation(out=junk[:], in_=x_sb[:, xb_, :], func=AF.Square, accum_out=ssq[:, t:t + 1]),
                   r=[bx[xb_]], w=[bjunk, bst[t]])
                op(dve, lambda t=t: V.tensor_scalar(out=ssq[:, t:t + 1], in0=ssq[:, t:t + 1], scalar1=1.0 / D, scalar2=EPS, op0=ALU.mult, op1=ALU.add),
                   w=[bst[t]])
                op(pool, lambda t=t: G.tensor_tensor(out=rstd[:, t:t + 1], in0=ssq[:, t:t + 1], in1=mhalf[:, 0:1], op=ALU.pow),
                   r=[bones], w=[bst[t]])
                yield
                xi = t % 2
                op(dve, lambda t=t, xi=xi, xb_=xb_: V.scalar_tensor_tensor(out=xn[xi][:], in0=x_sb[:, xb_, :], scalar=rstd[:, t:t + 1], in1=gmix[:],
                                                                          op0=ALU.mult, op1=ALU.mult),
                   r=[bx[xb_], bst[t], bconst], w=[bxn[xi]])
                yield
                bt, bb = kb.bank("B")
                btv = bt[:].bitcast(BF16).rearrange("p (k c) -> p k c", k=8)

                def tr8(xi=xi, btv=btv):
                    ins = None
                    for k in range(8):
                        ins = P_.transpose(out=btv[:, k, :], in_=xn[xi][:, k * 128:(k + 1) * 128], identity=identb[:])
                    return ins
                op(pe, tr8, r=[bxn[xi], bidb], w=[bb])
                op(act, lambda t=t, btv=btv: S.copy(out=xnT[:, :, t * 128:(t + 1) * 128], in_=btv), r=[bb], w=[bxnT[t]])
                yield

        def fm_chunk(q, wt, bw, wcol, m):
            bt, bb = kb.bank("A")

            def mm():
                ins = None
                for k in range(8):
                    ins = P_.matmul(bt[:], lhsT=wt[:, k, wcol:wcol + 128], rhs=xnT[:, k, :], start=(k == 0), stop=(k == 7))
                return ins
            op(pe, mm, r=[bw] + bxnT, w=[bb])
            bias = bcol[:, m:m + 1]
            if m < 4:
                c = m
                op(dve, lambda: V.scalar_tensor_tensor(out=qdT[:, c, :].rearrange("p (t i) -> p t i", t=4), in0=bview(bt, 4, 128), scalar=bias,
                                                       in1=qdtab[:, c * 128:(c + 1) * 128].unsqueeze(1).to_broadcast([128, 4, 128]),
                                                       op0=ALU.add, op1=ALU.mult), r=[bb, bbcol, bqd], pw=[bqdT])
            elif m < 8:
                c = m - 4
                op(dve, lambda: V.scalar_tensor_tensor(out=ksT[:, c, :].rearrange("p (t i) -> p t i", t=4), in0=bview(bt, 4, 128), scalar=bias,
                                                       in1=kstab[:, c * 128:(c + 1) * 128].unsqueeze(1).to_broadcast([128, 4, 128]),
                                                       op0=ALU.add, op1=ALU.mult), r=[bb, bbcol, bks], pw=[bksT])
            elif m < 12:
                op(dve, lambda: V.tensor_scalar(out=QT[:, m - 8, :], in0=bt[:], scalar1=bias, scalar2=None, op0=ALU.add), r=[bb, bbcol], pw=[bQT])
            elif m == 12:
                blk0 = q * 4
                op(dve, lambda: V.tensor_scalar(out=KT[:, blk0 * 128:blk0 * 128 + 512], in0=bt[:], scalar1=bias, scalar2=None, op0=ALU.add),
                   r=[bb, bbcol], w=bKT[blk0:blk0 + 4])
            elif m < 21:
                k_ = m - 13
                op(act, lambda: S.activation(out=ta[:, k_, :], in_=bt[:], func=AF.Tanh, bias=bhalf[:, m - 13:m - 12], scale=0.5), r=[bb, bbhalf], w=[bta[k_]])
            else:
                k_ = m - 21
                op(act, lambda: S.activation(out=tr_[:, k_, :], in_=bt[:], func=AF.Tanh, bias=bhalf[:, m - 13:m - 12], scale=0.5), r=[bb, bbhalf], w=[btr[k_]])

        FMCH = {0: (0, 0), 1: (1, 4), 6: (6, 8), 7: (7, 12), 8: (8, 13), 9: (9, 17), 10: (10, 21), 11: (11, 25)}

        def load_wch(ci):
            ensure_staged(ci)
            c0, n = WCH[ci]
            return load_w(Wb_v[:, :, c0:c0 + n], BWc[ci], n)

        def gen_fm(q, chunks):
            for ci in chunks:
                wt, bw = load_wch(ci)
                m = FMCH[ci][1]
                for cc in range(1 if ci == 7 else 4):
                    fm_chunk(q, wt, bw, cc * 128, m + cc)
                    yield
                if ci == 7:
                    blk0 = q * 4
                    for t in range(4):
                        bt, bb = kb.bank("A")

                        def mmav(t=t, bt=bt, wt=wt):
                            P_.matmul(bt[:, 0:128], lhsT=ones2[:], rhs=brow_b[:, 0:128], start=True, stop=False)
                            ins = None
                            for k in range(8):
                                ins = P_.matmul(bt[:, 0:128], lhsT=xnT[:, k, t * 128:(t + 1) * 128], rhs=wt[:, k, 128:256], start=False, stop=(k == 7))
                            return ins
                        op(pe, mmav, r=[bw, bxnT[t], bbrow, bones], w=[bb])
                        op(dve, lambda t=t, bt=bt: V.tensor_copy(out=VA[:, blk0 + t, :], in_=bt[:, 0:128]), r=[bb], w=[bVA[blk0 + t]])
                    yield

        def gen_tm(q, js=(0, 1, 2, 3)):
            for j in js:
                wt, bw = load_wch(2 + j)
                for t in range(4):
                    bt, bb = kb.bank("A")
                    bo = 128 + j * 512

                    def mmtm(t=t, bt=bt, wt=wt, bo=bo):
                        P_.matmul(bt[:], lhsT=ones2[:], rhs=brow_b[:, bo:bo + 512], start=True, stop=False)
                        ins = None
                        for k in range(8):
                            ins = P_.matmul(bt[:], lhsT=xnT[:, k, t * 128:(t + 1) * 128], rhs=wt[:, k, :], start=False, stop=(k == 7))
                        return ins
                    op(pe, mmtm, r=[bw, bxnT[t], bbrow, bones], w=[bb])
                    if j < 2:
                        op(act, lambda t=t, bt=bt, j=j: S.copy(out=Vr[t][:, j * 512:(j + 1) * 512], in_=bt[:]), r=[bb], pw=[bVr[t]])
                    else:
                        jj = j - 2
                        op(act, lambda bt=bt: S.activation(out=tg[:], in_=bt[:], func=AF.Tanh, scale=0.5), r=[bb], w=[btg])
                        op(dve, lambda t=t, bt=bt, jj=jj: V.scalar_tensor_tensor(out=sg[t][:, jj * 512:(jj + 1) * 512], in0=tg[:], scalar=1.0, in1=bt[:],
                                                                               op0=ALU.add, op1=ALU.mult), r=[bb, btg], pw=[bsg[t]])
                    yield

        def gen_att(q):
            def step1(t, g):
                blk = q * 4 + t
                pts = []
                for pc in ((0, 1) if blk > 0 else (1,)):
                    kblk = blk - 1 + pc
                    bt, bb = kb.bank("B")
                    op(pe, lambda bt=bt, kblk=kblk: P_.matmul(
                        bview(bt, 4, 128), lhsT=KT[64 * g:64 * g + 64, kblk * 128:(kblk + 1) * 128],
                        rhs=QT[64 * g:64 * g + 64, :, t * 128:(t + 1) * 128], start=True, stop=True),
                       r=[bKT[kblk], bQT], w=[bb])
                    ei = pc
                    op(act, lambda bt=bt, ei=ei: S.activation(out=eS[ei][:], in_=bt[:], func=AF.Exp, scale=0.125), r=[bb], w=[beS[ei]])
                    pi = 2 * g + pc
                    op(dve, lambda ei=ei, pi=pi, pc=pc: V.tensor_tensor(
                        out=PT[pi][:].rearrange("p (c i) -> p c i", c=4), in0=eS[ei][:].rearrange("p (c i) -> p c i", c=4),
                        in1=mtab4[:, pc, 4 * g:4 * g + 4, :], op=ALU.mult), r=[beS[ei], bmtab], w=[bPT[pi]])
                    pts.append((pi, kblk))
                return pts

            def step2(t, g, pts):
                btn, bbn = kb.bank("B")
                btd, bbd = kb.bank("B")

                def pv():
                    ins = None
                    last = len(pts) - 1
                    for hf in range(2):
                        for n_, (pi, kblk) in enumerate(pts):
                            P_.matmul(btn[64 * hf:64 * hf + 64, 0:256], lhsT=VA[:, kblk, 64 * g:64 * g + 64], rhs=PT[pi][:, 256 * hf:256 * hf + 256],
                                      start=(n_ == 0), stop=(n_ == last))
                    for hf in range(2):
                        for n_, (pi, kblk) in enumerate(pts):
                            ins = P_.matmul(btd[64 * hf:64 * hf + 64, 0:256], lhsT=onesb[:, 0:64], rhs=PT[pi][:, 256 * hf:256 * hf + 256],
                                            start=(n_ == 0), stop=(n_ == last))
                    return ins
                op(pe, pv, r=[bPT[pi] for pi, _ in pts] + [bVA[kb_] for _, kb_ in pts] + [bones], w=[bbn, bbd])
                op(dve, lambda: V.tensor_tensor(out=rden[:].rearrange("p (c i) -> p c i", c=2), in0=btd[:, 0:256].rearrange("p (c i) -> p c i", c=2),
                                                in1=esink[:, g, :, :], op=ALU.add), r=[bbd, bconst], w=[brden])
                op(dve, lambda: V.reciprocal(out=rden[:], in_=rden[:]), w=[brden])
                op(dve, lambda: V.tensor_tensor(out=attnT[:, g, :, t * 128:(t + 1) * 128], in0=btn[:, 0:256].rearrange("p (c i) -> p c i", c=2),
                                                in1=rden[:].rearrange("p (c i) -> p c i", c=2), op=ALU.mult),
                   r=[bbn, brden], pw=[battn[t]])

            pend = None
            for t in range(4):
                for g in range(2):
                    pts = step1(t, g)
                    yield
                    if pend is not None:
                        step2(*pend)
                        yield
                    pend = (t, g, pts)
            step2(*pend)
            yield

        def gen_ret(q):
            ybanks = {}

            def sa(t):
                tc0, tc1 = t * 128, (t + 1) * 128
                for p in range(2):
                    bt, bb = kb.bank("B")

                    def mmsc(bt=bt, p=p):
                        ins = None
                        for c in range(4):
                            ins = P_.matmul(bt[:, c * 128:(c + 1) * 128], lhsT=ksT[64 * p:64 * p + 64, c, tc0:tc1], rhs=qdT[64 * p:64 * p + 64, c, tc0:tc1],
                                            start=True, stop=True)
                        return ins
                    op(pe, mmsc, r=[bksT, bqdT], w=[bb])
                    op(dve, lambda bt=bt, p=p: V.tensor_tensor(out=scT[:, p, :, :], in0=bview(bt, 4, 128),
                                                              in1=caus[:].unsqueeze(1).to_broadcast([128, 4, 128]), op=ALU.mult),
                       r=[bb, bcaus], pw=[bscT])
                bt, bb = kb.bank("B")
                btv = bt[:].bitcast(BF16).rearrange("p (k c) -> p k c", k=8)

                def trk():
                    ins = None
                    for c in range(4):
                        ins = P_.transpose(out=btv[:, c, :], in_=ksT[:, c, tc0:tc1], identity=identb[:])
                    return ins
                op(pe, trk, r=[bksT, bidb], w=[bb])
                op(act, lambda: S.copy(out=kstok[:], in_=btv[:, 0:4, :]), r=[bb], w=[bkstok])

            def sbm(t):
                blk = q * 4 + t
                tc0, tc1 = t * 128, (t + 1) * 128
                if blk == 0:
                    op(dve, lambda: V.memset(state[:], 0.0), w=[bstate])
                    op(dve, lambda: V.memset(state_bf[0][:], 0.0), w=[bstbf[0]])
                sbi = blk % 2
                yb = []
                for p in range(2):
                    bt, bb = kb.bank("Y")

                    def mmy(bt=bt, p=p):
                        ins = None
                        for c in range(4):
                            h = 2 * c + p
                            P_.matmul(bt[:, c * 128:(c + 1) * 128], lhsT=scT[:, p, c, :], rhs=Vr[t][:, h * 128:(h + 1) * 128], start=True, stop=False)
                            ins = P_.matmul(bt[:, c * 128:(c + 1) * 128], lhsT=qdT[64 * p:64 * p + 64, c, tc0:tc1],
                                            rhs=state_bf[sbi][64 * p:64 * p + 64, c * 128:(c + 1) * 128], start=False, stop=True)
                        return ins
                    op(pe, mmy, r=[bscT, bVr[t], bqdT, bstbf[sbi]], w=[bb])
                    yb.append((bt, bb))
                ybanks[t] = yb
                btk, bbk = kb.bank("B")

                def mmkv():
                    ins = None
                    for c in range(4):
                        for p in range(2):
                            h = 2 * c + p
                            ins = P_.matmul(btk[64 * p:64 * p + 64, c * 128:(c + 1) * 128], lhsT=kstok[:, c, 64 * p:64 * p + 64],
                                            rhs=Vr[t][:, h * 128:(h + 1) * 128], start=True, stop=True)
                    return ins
                op(pe, mmkv, r=[bkstok, bVr[t]], w=[bbk])
                op(dve, lambda: V.tensor_tensor(out=stmp[:], in0=btk[:], in1=state[:], op=ALU.add), r=[bbk, bstate], w=[bstmp])
                op(dve, lambda: V.tensor_tensor(out=state[:], in0=stmp[:], in1=gctab[:], op=ALU.mult), r=[bstmp, bgc], w=[bstate])
                op(pool, lambda: G.tensor_copy(out=state_bf[1 - sbi][:], in_=state[:]), r=[bstate], w=[bstbf[1 - sbi]])

            def sc(t):
                st8, bst8 = st8_2[t % 2], bst8_2[t % 2]
                for p, (bt, bb) in enumerate(ybanks[t]):
                    op(dve, lambda bt=bt, p=p: V.tensor_reduce(out=st8[:, 0, 4 * p:4 * p + 4], in_=bview(bt, 4, 128), axis=AX.X, op=ALU.add), r=[bb], pw=[bst8])
                    op(act, lambda bt=bt, p=p: S.activation(out=ysq[:, 512 * p:512 * (p + 1)], in_=bt[:], func=AF.Square), r=[bb], pw=[bysq])
                op(dve, lambda: V.tensor_reduce(out=st8[:, 1, :], in_=ysq[:].rearrange("p (h e) -> p h e", h=8), axis=AX.X, op=ALU.add), r=[bysq], pw=[bst8])
                op(dve, lambda: V.scalar_tensor_tensor(out=st8[:, 3, :], in0=st8[:, 0, :], scalar=1.0 / (128.0 * 128.0), in1=st8[:, 0, :], op0=ALU.mult, op1=ALU.mult), w=[bst8])
                op(dve, lambda: V.scalar_tensor_tensor(out=st8[:, 4, :], in0=st8[:, 1, :], scalar=1.0 / 128, in1=st8[:, 3, :], op0=ALU.mult, op1=ALU.subtract), w=[bst8])
                op(pool, lambda: G.tensor_scalar(out=st8[:, 4, :], in0=st8[:, 4, :], scalar1=4.0, scalar2=4.0 * EPS, op0=ALU.mult, op1=ALU.add), w=[bst8])
                op(pool, lambda: G.tensor_tensor(out=st8[:, 6, :], in0=st8[:, 4, :], in1=mhalf[:], op=ALU.pow), r=[bones], w=[bst8])
                op(dve, lambda: V.scalar_tensor_tensor(out=st8[:, 7, :], in0=st8[:, 0, :], scalar=-1.0 / 128, in1=st8[:, 6, :], op0=ALU.mult, op1=ALU.mult), w=[bst8])

            def sd(t):
                st8, bst8 = st8_2[t % 2], bst8_2[t % 2]
                for p, (bt, bb) in enumerate(ybanks[t]):
                    def nrm(bt=bt, p=p):
                        ins = None
                        for c in range(4):
                            h = 2 * c + p
                            si = 4 * p + c
                            ins = S.activation(out=yn[:, h * 128:(h + 1) * 128], in_=bt[:, c * 128:(c + 1) * 128], func=AF.Identity,
                                               scale=st8[:, 6, si:si + 1], bias=st8[:, 7, si:si + 1])
                        return ins
                    op(act, nrm, r=[bb, bst8], pw=[byn])
                op(dve, lambda: V.tensor_tensor(out=retg[:], in0=yn[:], in1=sg[t][:], op=ALU.mult), r=[byn, bsg[t]], w=[bretg])
                yield
                bt, bb = kb.bank("B")
                btv = bt[:].bitcast(BF16).rearrange("p (k c) -> p k c", k=8)

                def trr():
                    ins = None
                    for k in range(8):
                        ins = P_.transpose(out=btv[:, k, :], in_=retg[:, k * 128:(k + 1) * 128], identity=identb[:])
                    return ins
                op(pe, trr, r=[bretg, bidb], w=[bb])
                op(act, lambda: S.copy(out=retgT[:, :, t * 128:(t + 1) * 128], in_=btv), r=[bb], w=[bretgT[t]])

            sa(0); yield
            sbm(0); yield
            sc(0); yield
            for t in range(1, 4):
                sa(t); yield
                sbm(t); yield
                sc(t); yield
                yield from sd(t - 1); yield
            yield from sd(3); yield

        def gen_up(q):
            ensure_staged("wau")
            ensure_staged("wru")
            wa, bwa = load_w(Waub, bWau)
            wr_ = [load_w(Wru_v[:, :, j * 512:(j + 1) * 512], bWru) for j in range(2)]
            for dm in range(8):
                bta_, bba = kb.bank("A")
                btr2, bbr = kb.bank("A")
                j, dc = dm // 4, (dm % 4) * 128

                def mma(bta_=bta_, j=j, dc=dc):
                    ins = None
                    for kk in range(4):
                        ins = P_.matmul(bta_[:], lhsT=wa[:, 4 * j + kk, dc:dc + 128], rhs=attnT[:, kk // 2, kk % 2, :], start=(kk == 0), stop=(kk == 3))
                    return ins

                def mmr(btr2=btr2, j=j, dc=dc):
                    ins = None
                    for k in range(8):
                        ins = P_.matmul(btr2[:], lhsT=wr_[j][0][:, k, dc:dc + 128], rhs=retgT[:, k, :], start=(k == 0), stop=(k == 7))
                    return ins
                op(pe, mma, r=battn + [bwa], w=[bba])
                op(pe, mmr, r=bretgT + [wr_[j][1]], w=[bbr])
                op(dve, lambda bta_=bta_, dm=dm: V.scalar_tensor_tensor(out=m1[:], in0=ta[:, dm, :], scalar=1.0, in1=bta_[:], op0=ALU.add, op1=ALU.mult),
                   r=[bba, bta[dm]], w=[bm1])
                op(dve, lambda btr2=btr2, dm=dm: V.scalar_tensor_tensor(out=m2[:], in0=tr_[:, dm, :], scalar=1.0, in1=btr2[:], op0=ALU.add, op1=ALU.mult),
                   r=[bbr, btr[dm]], w=[bm2])
                op(pool, lambda dm=dm: G.tensor_tensor(out=mixT[:, dm, :], in0=m1[:], in1=m2[:], op=ALU.add), r=[bm1, bm2], w=[bmix[dm]])
                yield

        def gen_out(s, q, routes):
            tok0 = s * SEQ + q * 512
            ensure_staged("wout")
            wo = [load_w(Wout_v[:, :, j * 512:(j + 1) * 512], bWout) for j in range(2)]
            for t in range(4):
                xi = t % 2
                tokr = slice(tok0 + t * 128, tok0 + (t + 1) * 128)
                op(sp, lambda xi=xi, tokr=tokr: nc.sync.dma_start(out=x2[xi][:], in_=x[tokr, :]), w=[bx2[xi]], q=qsp)
                for hf in range(2):
                    bt, bb = kb.bank("A")

                    def mmo(bt=bt, t=t, hf=hf):
                        ins = None
                        for k in range(8):
                            ins = P_.matmul(bt[:], lhsT=mixT[:, k, t * 128:(t + 1) * 128], rhs=wo[hf][0][:, k, :], start=(k == 0), stop=(k == 7))
                        return ins
                    op(pe, mmo, r=bmix + [wo[hf][1]], w=[bb])
                    op(dve, lambda bt=bt, hf=hf, xi=xi: V.scalar_tensor_tensor(out=x2[xi][:, hf * 512:(hf + 1) * 512], in0=bt[:], scalar=0.5,
                                                                                in1=x2[xi][:, hf * 512:(hf + 1) * 512], op0=ALU.mult, op1=ALU.add),
                       r=[bb], w=[bx2[xi]])
                    yield
                op(sp, lambda xi=xi, tokr=tokr: nc.sync.dma_start(out=X2[tokr, :], in_=x2[xi][:]), r=[bx2[xi]], pw=[bX2], q=qsp)
                if "x2" in taps:
                    if "x2" not in tapo:
                        tapo["x2"] = nc.dram_tensor("tap_x2", [T, D], F32, kind="ExternalOutput").ap()
                    op(sp, lambda xi=xi, tokr=tokr: nc.sync.dma_start(out=tapo["x2"][tokr, :], in_=x2[xi][:]), r=[bx2[xi]], q=qsp)
                if stop_after != "mixer":
                    drain_bg()
                    ensure_zero()
                    route_a((tok0 // 128) + t, xi)
                    bgs.append(route_b((tok0 // 128) + t, xi))

        bgs = []

        def step_all(lst):
            for b in list(lst):
                try:
                    next(b)
                except StopIteration:
                    lst.remove(b)

        def drain_bg():
            while bgs:
                step_all(bgs)

        def run_a(a, bs):
            for _ in a:
                step_all(bs)
                step_all(bgs)
                stage_tick()
                zero_tick()

        def drain(bs):
            while bs:
                step_all(bs)
                step_all(bgs)

        def chain(*gens):
            for g_ in gens:
                yield from g_

        groups = [(s_, q_) for s_ in range(nseq) for q_ in range(4)]
        if "nogroups" in taps:
            groups = []
        elif "onegroup" in taps:
            groups = groups[:1]
        for _ in (gen_norm(*groups[0]) if groups else ()):
            pass
        for gi, (s_, q_) in enumerate(groups):
            bs = []
            if "e1" in taps:
                run_a(chain(gen_fm(q_, [0, 1]), gen_tm(q_), gen_fm(q_, [6, 7, 8, 9, 10, 11])), bs)
                bs.append(gen_ret(q_))
                bs.append(gen_att(q_))
                drain(bs)
            elif "e6" in taps:
                run_a(chain(gen_fm(q_, [6, 7]), gen_fm(q_, [0, 1]), gen_tm(q_)), bs)
                bs.append(gen_att(q_))
                run_a(gen_fm(q_, [8, 9, 10, 11]), bs)
                drain(bs)
                bs.append(gen_ret(q_))
                drain(bs)
            elif "e7" in taps:
                run_a(chain(gen_fm(q_, [0, 1]), gen_tm(q_)), bs)
                bs.append(gen_ret(q_))
                run_a(gen_fm(q_, [6, 7]), bs)
                drain(bs)
                bs.append(gen_att(q_))
                drain(bs)
                run_a(gen_fm(q_, [8, 9, 10, 11]), bs)
            elif "e3" in taps:
                run_a(chain(gen_fm(q_, [0, 1]), gen_tm(q_)), bs)
                bs.append(gen_ret(q_))
                run_a(gen_fm(q_, [6, 7]), bs)
                drain(bs)
                bs.append(gen_att(q_))
                run_a(gen_fm(q_, [8, 9, 10, 11]), bs)
                drain(bs)
            elif "e4" in taps:
                run_a(chain(gen_fm(q_, [0, 1]), gen_tm(q_), gen_fm(q_, [6, 7])), bs)
                bs.append(gen_ret(q_))
                bs.append(gen_att(q_))
                run_a(gen_fm(q_, [8, 9, 10, 11]), bs)
                drain(bs)
            elif "e0" not in taps:
                kb.set_pools([0, 1, 4, 5], [2, 3, 6, 7])
                run_a(gen_fm(q_, [6, 7]), bs)
                bs.append(gen_att(q_))
                run_a(chain(gen_fm(q_, [0, 1]), gen_tm(q_)), bs)
                drain(bs)
                kb.set_pools([0, 1], [2, 3], [4, 5, 6, 7])
                bs.append(gen_ret(q_))
                run_a(gen_fm(q_, [8, 9, 10, 11]), bs)
                drain(bs)
                kb.set_pools([0, 1, 4, 5, 6, 7], [2, 3])
            else:
                run_a(chain(gen_fm(q_, [0, 1]), gen_tm(q_)), bs)
                bs.append(gen_ret(q_))
                if "seq" in taps:
                    drain(bs)
                run_a(gen_fm(q_, [6, 7]), bs)
                bs.append(gen_att(q_))
                if "seq" in taps:
                    drain(bs)
                run_a(gen_fm(q_, [8, 9, 10, 11]), bs)
                drain(bs)
            if gi + 1 < len(groups):
                bs.append(gen_norm(*groups[gi + 1]))
            run_a(chain(gen_up(q_), gen_out(s_, q_, None)), bs)
            drain(bs)
        drain_bg()
        if "dest" in taps:
            tapo["dest"] = nc.dram_tensor("tap_dest", [128, NT, 2], I32, kind="ExternalOutput").ap()
            op(sp, lambda: nc.sync.dma_start(out=tapo["dest"], in_=dest_all[:]), r=[bdest], q=qsp)
            tapo["wts"] = nc.dram_tensor("tap_wts", [128, NT, 2], F32, kind="ExternalOutput").ap()
            op(sp, lambda: nc.sync.dma_start(out=tapo["wts"], in_=wts[:]), r=[bwts], q=qsp)

        if stop_after is None:
            kb.barrier()
            es_mix.close()
            es_moe = ExitStack()
            kb.cur = es_moe
            NSL = CAP // 128
            wg = [sb(f"wg{i}", [128, 8, DE], BF16) for i in range(2)]
            wu = [sb(f"wu{i}", [128, 8, DE], BF16) for i in range(2)]
            wd = [sb(f"wd{i}", [128, 4, D], BF16) for i in range(2)]
            bwg, bwu, bwd = [Buf(), Buf()], [Buf(), Buf()], [Buf(), Buf()]
            Xe = [sb(f"Xe{i}", [128, NSL, D], BF16) for i in range(2)]
            bXe = [Buf(), Buf()]
            XT = [sb(f"XT{i}", [128, 8, CAP], BF16) for i in range(2)]
            bXT = [[Buf() for _ in range(NSL)] for _ in range(2)]
            tge = [sb(f"tge{i}", [128, CAP], F32) for i in range(2)]
            btge = [Buf(), Buf()]
            sgu = [sb(f"sgu{i}", [128, CAP], F32) for i in range(2)]
            bsgu = [Buf(), Buf()]
            actT = [sb(f"actT{i}", [128, 4, CAP], BF16) for i in range(2)]
            bactT = [[Buf() for _ in range(4)] for _ in range(2)]
            Yt = [sb(f"Yt{i}", [128, D], BF16) for i in range(2)]
            bYt = [Buf(), Buf()]
            yti = [0]

            def load_expert(e):
                i = e % 2
                op(pool, lambda: G.dma_start(out=wg[i][:], in_=w_ge[e].rearrange("(k p) f -> p k f", p=128)), w=[bwg[i]], q=qpl)
                op(pool, lambda: G.dma_start(out=wu[i][:], in_=w_ue[e].rearrange("(k p) f -> p k f", p=128)), w=[bwu[i]], q=qpl)
                op(pool, lambda: G.dma_start(out=wd[i][:], in_=w_de[e].rearrange("(k p) n -> p k n", p=128)), w=[bwd[i]], q=qpl)
                op(sp, lambda: nc.sync.dma_start(out=Xe[i][:], in_=Xg[e * CAP:(e + 1) * CAP, :].rearrange("(s p) d -> p s d", p=128)),
                   r=[bXg], w=[bXe[i]], q=qsp)

            def transpose_slots(e):
                i = e % 2
                for sl in range(NSL):
                    bt, bb = kb.bank()
                    btv = bt[:].bitcast(BF16).rearrange("p (k c) -> p k c", k=8)

                    def trx(btv=btv, sl=sl):
                        ins = None
                        for k in range(8):
                            ins = P_.transpose(out=btv[:, k, :], in_=Xe[i][:, sl, k * 128:(k + 1) * 128], identity=identb[:])
                        return ins
                    op(pe, trx, r=[bXe[i], bidb], w=[bb])
                    if sl % 2 == 0:
                        op(act, lambda btv=btv, sl=sl: S.copy(out=XT[i][:, :, sl * 128:(sl + 1) * 128], in_=btv), r=[bb], w=[bXT[i][sl]])
                    else:
                        op(dve, lambda btv=btv, sl=sl: V.tensor_copy(out=XT[i][:, :, sl * 128:(sl + 1) * 128], in_=btv), r=[bb], w=[bXT[i][sl]])

            load_expert(0)
            transpose_slots(0)
            for e in range(NE):
                i = e % 2
                if e + 1 < NE:
                    load_expert(e + 1)
                for f in range(4):
                    btg_, bbg = kb.bank()
                    btu, bbu = kb.bank()

                    def mmg(btg_=btg_, f=f):
                        ins = None
                        for k in range(8):
                            ins = P_.matmul(btg_[:, 0:CAP], lhsT=wg[i][:, k, f * 128:(f + 1) * 128], rhs=XT[i][:, k, :], start=(k == 0), stop=(k == 7))
                        return ins

                    def mmu(btu=btu, f=f):
                        ins = None
                        for k in range(8):
                            ins = P_.matmul(btu[:, 0:CAP], lhsT=wu[i][:, k, f * 128:(f + 1) * 128], rhs=XT[i][:, k, :], start=(k == 0), stop=(k == 7))
                        return ins
                    op(pe, mmg, r=[bwg[i]] + bXT[i], w=[bbg])
                    op(pe, mmu, r=[bwu[i]] + bXT[i], w=[bbu])
                    fi = f % 2
                    op(act, lambda btg_=btg_, fi=fi: S.activation(out=tge[fi][:], in_=btg_[:, 0:CAP], func=AF.Tanh, scale=0.5), r=[bbg], w=[btge[fi]])
                    op(dve, lambda btg_=btg_, fi=fi: V.scalar_tensor_tensor(out=sgu[fi][:], in0=tge[fi][:], scalar=1.0, in1=btg_[:, 0:CAP], op0=ALU.add, op1=ALU.mult),
                       r=[bbg, btge[fi]], w=[bsgu[fi]])
                    op(dve, lambda btu=btu, fi=fi, f=f: V.tensor_tensor(out=actT[i][:, f, :], in0=sgu[fi][:], in1=btu[:, 0:CAP], op=ALU.mult),
                       r=[bbu, bsgu[fi]], w=[bactT[i][f]])
                if e + 1 < NE:
                    transpose_slots(e + 1)
                for sl in range(NSL):
                    yi = yti[0] % 2
                    yti[0] += 1
                    for hf in range(2):
                        bt, bb = kb.bank()

                        def mmd(bt=bt, sl=sl, hf=hf):
                            ins = None
                            for f in range(4):
                                ins = P_.matmul(bt[:], lhsT=actT[i][:, f, sl * 128:(sl + 1) * 128], rhs=wd[i][:, f, hf * 512:(hf + 1) * 512], start=(f == 0), stop=(f == 3))
                            return ins
                        op(pe, mmd, r=bactT[i] + [bwd[i]], w=[bb])
                        if hf == 0:
                            op(act, lambda bt=bt, yi=yi: S.copy(out=Yt[yi][:, 0:512], in_=bt[:]), r=[bb], pw=[bYt[yi]])
                        else:
                            op(dve, lambda bt=bt, yi=yi: V.tensor_copy(out=Yt[yi][:, 512:1024], in_=bt[:]), r=[bb], pw=[bYt[yi]])
                    r0 = e * CAP + sl * 128
                    op(sp, lambda yi=yi, r0=r0: nc.sync.dma_start(out=Yg[r0:r0 + 128, :], in_=Yt[yi][:]), r=[bYt[yi]], pw=[bYg], q=qsp)

            kb.barrier()
            es_moe.close()
            es_fin = ExitStack()
            kb.cur = es_fin
            NB_ = 4
            y1 = [sb(f"y1_{i}", [128, D], BF16) for i in range(NB_)]
            y2 = [sb(f"y2_{i}", [128, D], BF16) for i in range(NB_)]
            xr = [sb(f"xr{i}", [128, D], F32) for i in range(NB_)]
            by1, by2, bxr = [Buf() for _ in range(NB_)], [Buf() for _ in range(NB_)], [Buf() for _ in range(NB_)]
            ot = [sb(f"ot{i}", [128, D], F32) for i in range(2)]
            bot = [Buf(), Buf()]
            y1s = [sb(f"y1s{i}", [128, D], F32) for i in range(2)]
            by1s = [Buf(), Buf()]
            jk = sb("jk", [128, D], BF16)
            bjk = Buf()
            fs = sb("fs", [128, NT, 2], F32)
            bfs = [Buf() for _ in range(NT)]

            def fin_load(tt):
                i = tt % NB_
                op(pool, lambda: G.indirect_dma_start(out=y1[i][:], out_offset=None, in_=Yg[:, :],
                                                      in_offset=bass.IndirectOffsetOnAxis(ap=dest_all[:, tt, 0:1], axis=0),
                                                      bounds_check=breg2, oob_is_err=False), r=[bYg, bdest], w=[by1[i]], q=qpl)
                op(pool, lambda: G.indirect_dma_start(out=y2[i][:], out_offset=None, in_=Yg[:, :],
                                                      in_offset=bass.IndirectOffsetOnAxis(ap=dest_all[:, tt, 1:2], axis=0),
                                                      bounds_check=breg2, oob_is_err=False), r=[bYg, bdest], w=[by2[i]], q=qpl)
                op(sp, lambda: nc.sync.dma_start(out=xr[i][:], in_=X2[tt * 128:(tt + 1) * 128, :]), r=[bX2], w=[bxr[i]], q=qsp)

            def fin_a(tt):
                i = tt % NB_
                k_ = tt % 2
                op(act, lambda: S.activation(out=y1s[k_][:], in_=y1[i][:], func=AF.Copy, scale=wts[:, tt, 0:1]), r=[bwts, by1[i]], w=[by1s[k_]])
                op(dve, lambda: V.scalar_tensor_tensor(out=xr[i][:], in0=y2[i][:], scalar=wts[:, tt, 1:2], in1=xr[i][:], op0=ALU.mult, op1=ALU.add),
                   r=[by2[i], bwts], w=[bxr[i]])
                op(dve, lambda: V.tensor_tensor(out=xr[i][:], in0=xr[i][:], in1=y1s[k_][:], op=ALU.add), r=[by1s[k_]], w=[bxr[i]])
                op(act, lambda: S.activation(out=jk[:], in_=xr[i][:], func=AF.Square, accum_out=fs[:, tt, 0:1]), r=[bxr[i]], w=[bjk, bfs[tt]])
                op(dve, lambda: V.tensor_scalar(out=fs[:, tt, 0:1], in0=fs[:, tt, 0:1], scalar1=1.0 / D, scalar2=EPS, op0=ALU.mult, op1=ALU.add), w=[bfs[tt]])
                op(pool, lambda: G.tensor_tensor(out=fs[:, tt, 1:2], in0=fs[:, tt, 0:1], in1=mhalf[:, 0:1], op=ALU.pow), r=[bones], w=[bfs[tt]])

            def fin_b(tt):
                i = tt % NB_
                o = tt % 2
                op(act, lambda: S.activation(out=ot[o][:], in_=xr[i][:], func=AF.Copy, scale=fs[:, tt, 1:2]), r=[bxr[i], bfs[tt]], w=[bot[o]])
                op(dve, lambda: V.tensor_tensor(out=ot[o][:], in0=ot[o][:], in1=gfin[:], op=ALU.mult), r=[bgfin], w=[bot[o]])
                op(sp, lambda: nc.sync.dma_start(out=out[tt * 128:(tt + 1) * 128, :], in_=ot[o][:]), r=[bot[o]], q=qsp)

            for tt in range(min(3, NT)):
                fin_load(tt)
            for tt in range(NT):
                fin_a(tt)
                if tt >= 1:
                    fin_b(tt - 1)
                if tt + 3 < NT:
                    fin_load(tt + 3)
            fin_b(NT - 1)

        for qq in (kb.qsp, kb.qpl, kb.qact, kb.qcast):
            for sem, c in zip(qq.sems, qq.cnt):
                if c:
                    sp.wait(sem, 16 * c)
        for E in (pe, act, dve, pool):
            if E.sem is not None:
                sp.wait(E.sem, E.cnt)
        if kb.cur is not es:
            kb.cur.close()
    return nc


def kernel(**inputs):
    n = N_CORES
    nseq = inputs["x"].shape[0] // n
    nc = build_nc(nseq=nseq)
    consts = host_consts()
    shared = {}
    for k in ("norm_mix_g", "w_in", "b_in", "attn_sinks", "w_attn_up", "w_ret_up", "w_out", "norm_ffn_g", "w_group_router",
              "b_group_router", "w_expert_router", "b_expert_router", "w_gate_e", "w_up_e", "w_down_e"):
        a = np.asarray(inputs[k])[0]
        shared[k] = np.ascontiguousarray(a.reshape(1, -1) if a.ndim == 1 else a, dtype=np.float32)
    shared["norm_final_g"] = np.ascontiguousarray(np.asarray(inputs["norm_final_g"]).reshape(1, -1), dtype=np.float32)
    shared.update(consts)
    x = np.asarray(inputs["x"], dtype=np.float32)
    in_maps = []
    for c in range(n):
        m = dict(shared)
        m["x"] = np.ascontiguousarray(x[c * nseq:(c + 1) * nseq].reshape(nseq * SEQ, D))
        in_maps.append(m)
    res = run_bass_kernel_spmd(nc, in_maps, core_ids=list(range(n)))
    outs = [np.asarray(r["out"]).reshape(nseq, SEQ, D) for r in res.results]
    return np.concatenate(outs, axis=0).astype(np.float32)
```

```python
import numpy as np
from contextlib import ExitStack
import concourse.bass as bass
import concourse.mybir as mybir
from concourse.bass_utils import run_bass_kernel_spmd

F32 = mybir.dt.float32
BF16 = mybir.dt.bfloat16
I32 = mybir.dt.int32
U32 = mybir.dt.uint32
AF = mybir.ActivationFunctionType
ALU = mybir.AluOpType
AX = mybir.AxisListType

D = 1024
SEQ = 2048
CH = 128
NBLK = SEQ // CH
IN_W = 5888
EPS = 1e-6
N_CORES = 8
NE = 32
DE = 512
CAP = 384
NSLOT = NE * CAP
OOB = NSLOT


class Sem:
    __slots__ = ("h", "name")

    def __init__(self, h, name):
        self.h = h
        self.name = name


class Eng:
    def __init__(self, kb, name, eng):
        self.kb = kb
        self.name = name
        self.eng = eng
        self.sem = None
        self.cnt = 0
        self.nsem = 0
        self.seen = {}

    def wait(self, sem, val):
        if self.seen.get(sem, 0) >= val:
            return
        self.eng.wait_ge(sem.h, val)
        self.seen[sem] = val

    def mark(self, instr):
        if self.sem is None or self.cnt >= 30000:
            self.sem = self.kb.newsem(f"{self.name}{self.nsem}")
            self.nsem += 1
            self.cnt = 0
        self.cnt += 1
        instr.then_inc(self.sem.h, 1)
        return (self.sem, self.cnt)


class DmaQ:
    def __init__(self, kb, E, n, name):
        self.E = E
        self.sems = [kb.newsem(f"{name}{i}") for i in range(n)]
        self.cnt = [0] * n
        self.i = 0

    def issue(self, fn):
        j = self.i % len(self.sems)
        self.i += 1
        sem = self.sems[j]
        if self.cnt[j] > 0:
            self.E.wait(sem, 16 * self.cnt[j])
        instr = fn()
        self.cnt[j] += 1
        instr.then_inc(sem.h, 16)
        return (sem, 16 * self.cnt[j])


class Buf:
    __slots__ = ("ws", "rs", "name")

    def __init__(self, name=""):
        self.ws = {}
        self.rs = {}
        self.name = name


class KB:
    def __init__(self, nc, es):
        self.nc = nc
        self.es = es
        self.cur = es
        self.pe = Eng(self, "pe", nc.tensor)
        self.act = Eng(self, "act", nc.scalar)
        self.dve = Eng(self, "dve", nc.vector)
        self.pool = Eng(self, "pool", nc.gpsimd)
        self.sp = Eng(self, "sp", nc.sync)
        self.qsp = DmaQ(self, self.sp, 20, "qsp")
        self.qpl = DmaQ(self, self.pool, 12, "qpl")
        self.qact = DmaQ(self, self.act, 6, "qact")
        self.banks = []
        self.bi = 0
        self.set_pools([0, 1], [2, 3], [4, 5, 6, 7])

    def newsem(self, name):
        return Sem(self.es.enter_context(self.nc.semaphore(name)), name)

    def sb(self, name, shape, dt):
        return self.cur.enter_context(self.nc.sbuf_tensor(name, shape, dt))

    def barrier(self):
        engs = (self.pe, self.act, self.dve, self.pool, self.sp)
        for E in engs:
            for E2 in engs:
                if E2 is not E and E2.sem is not None:
                    E.wait(E2.sem, E2.cnt)
            for qq in (self.qsp, self.qpl, self.qact) + ((self.qcast,) if hasattr(self, "qcast") else ()):
                for sem, c in zip(qq.sems, qq.cnt):
                    if c:
                        E.wait(sem, 16 * c)

    def op(self, E, fn, r=(), w=(), pw=(), q=None):
        need = {}

        def addall(d):
            for s, v in d.items():
                if need.get(s, 0) < v:
                    need[s] = v

        for b in r:
            addall(b.ws)
        for b in w:
            addall(b.ws)
            addall(b.rs)
        for b in pw:
            addall(b.rs)
        for s, v in need.items():
            E.wait(s, v)
        if q is None:
            ev = E.mark(fn())
        else:
            ev = q.issue(fn)
        s, v = ev
        for b in r:
            if b.rs.get(s, 0) < v:
                b.rs[s] = v
        for b in w:
            b.ws = {s: v}
            b.rs = {}
        for b in pw:
            if b.ws.get(s, 0) < v:
                b.ws[s] = v
        return ev

    def set_pools(self, A, B, Y=()):
        self.pools = {"A": list(A), "B": list(B), "Y": list(Y)}
        self.pctr = {"A": 0, "B": 0, "Y": 0}

    def bank(self, pool=None):
        if pool is None:
            b = self.banks[self.bi % len(self.banks)]
            self.bi += 1
            return b
        lst = self.pools[pool]
        b = self.banks[lst[self.pctr[pool] % len(lst)]]
        self.pctr[pool] += 1
        return b


def host_consts():
    j = np.arange(128)[:, None].astype(np.float64)
    i = np.arange(128)[None, :].astype(np.float64)
    slopes = 2.0 ** (-8.0 * np.arange(1, 9) / 8.0)
    mt = np.zeros((128, 2, 8, 128), np.float64)
    for h in range(8):
        dc = i - j
        mt[:, 1, h, :] = np.where(dc >= 0, np.exp(-slopes[h] * np.maximum(dc, 0)), 0.0)
        dp = i + 128 - j
        mt[:, 0, h, :] = np.where(dp < 128, np.exp(-slopes[h] * dp), 0.0)
    caus = (j <= i).astype(np.float64)
    gam = 1.0 - 2.0 ** (-5.0 - np.arange(8))
    qd = np.zeros((128, 4, 128))
    ks = np.zeros((128, 4, 128))
    gc = np.zeros((128, 4, 128))
    pos = np.arange(128)
    for c in range(4):
        for p in range(2):
            g = gam[2 * c + p]
            qd[64 * p:64 * p + 64, c, :] = (g ** (pos + 1.0))[None, :]
            ks[64 * p:64 * p + 64, c, :] = (g ** (-(pos + 1.0)) / 8.0)[None, :]
            gc[64 * p:64 * p + 64, c, :] = g ** 128.0
    ident = np.eye(128)
    tri = (j < i).astype(np.float64)
    ebase = np.tile((np.arange(NE) * CAP)[None, :], (128, 1))
    c = {
        "c_mtab": mt.reshape(128, -1), "c_caus": caus, "c_qd": qd.reshape(128, -1),
        "c_ks": ks.reshape(128, -1), "c_gc": gc.reshape(128, -1), "c_ident": ident,
        "c_tri": tri, "c_ebase": ebase,
    }
    return {k: np.ascontiguousarray(v, dtype=np.float32) for k, v in c.items()}


def build_nc(nseq=2, taps=(), stop_after=None):
    T = nseq * SEQ
    NT = T // 128
    nc = bass.Bass("TRN2", target_bir_lowering=False)

    def din(name, shape, dt=F32):
        return nc.dram_tensor(name, list(shape), dt, kind="ExternalInput").ap()

    def dint(name, shape, dt):
        return nc.dram_tensor(name, list(shape), dt, kind="Internal").ap()

    x = din("x", [T, D])
    norm_mix_g = din("norm_mix_g", [1, D])
    w_in = din("w_in", [D, IN_W])
    b_in = din("b_in", [1, IN_W])
    sinks = din("attn_sinks", [1, 8])
    w_au = din("w_attn_up", [512, D])
    w_ru = din("w_ret_up", [D, D])
    w_out = din("w_out", [D, D])
    norm_ffn_g = din("norm_ffn_g", [1, D])
    w_gr = din("w_group_router", [D, 4])
    b_gr = din("b_group_router", [1, 4])
    w_er = din("w_expert_router", [D, NE])
    b_er = din("b_expert_router", [1, NE])
    w_ge = din("w_gate_e", [NE, D, DE])
    w_ue = din("w_up_e", [NE, D, DE])
    w_de = din("w_down_e", [NE, DE, D])
    norm_fin_g = din("norm_final_g", [1, D])
    hc = host_consts()
    cin = {k: din(k, v.shape) for k, v in hc.items()}
    out = nc.dram_tensor("out", [T, D], F32, kind="ExternalOutput").ap()
    tapo = {}

    Wb = dint("Wb", [D, IN_W], BF16)
    Waub = dint("Waub", [128, 8, 512], BF16)
    Wrub = dint("Wrub", [D, D], BF16)
    Woutb = dint("Woutb", [D, D], BF16)
    X2 = dint("X2", [T, D], F32)
    Xg = dint("Xg", [NSLOT, D], BF16)
    Yg = dint("Yg", [NSLOT + 128, D], BF16)

    es = ExitStack()
    with es:
        kb = KB(nc, es)
        op = kb.op
        pe, act, dve, pool, sp = kb.pe, kb.act, kb.dve, kb.pool, kb.sp
        qsp, qpl = kb.qsp, kb.qpl
        V, S, G, P_ = nc.vector, nc.scalar, nc.gpsimd, nc.tensor
        sb = kb.sb
        for i in range(8):
            kb.banks.append((es.enter_context(nc.psum_tensor(f"bank{i}", [128, 512], F32)), Buf(f"bank{i}")))

        def tap(name, src_ap, shape, rbufs, dt=F32):
            if name not in taps:
                return
            if name not in tapo:
                tapo[name] = nc.dram_tensor("tap_" + name, list(shape), dt, kind="ExternalOutput").ap()
            return tapo[name]

        bWau, bWru, bWout = Buf(), Buf(), Buf()
        SRC = {"aq": 0, "ak": 512, "av": 640, "rq": 768, "rk": 1280, "rv": 1792, "rg": 2816, "ga": 3840, "gr": 4864}
        DST = {"rq": 0, "rk": 512, "rv": 1024, "rg": 2048, "aq": 3072, "ak": 3584, "av": 3712, "ga": 3840, "gr": 4864}
        WID = {"aq": 512, "ak": 128, "av": 128, "rq": 512, "rk": 512, "rv": 1024, "rg": 1024, "ga": 1024, "gr": 1024}
        WCH = [(0, 512), (512, 512), (1024, 512), (1536, 512), (2048, 512), (2560, 512), (3072, 512), (3584, 256),
               (3840, 512), (4352, 512), (4864, 512), (5376, 512)]
        BWc = [Buf(f"Wb{i}") for i in range(len(WCH))]

        def chunk_of(col):
            for i, (c0, n) in enumerate(WCH):
                if c0 <= col < c0 + n:
                    return i


        def ctile(name, key, shape, dt=F32, q=None):
            t = sb(name, shape, dt)
            b = Buf(name)
            if dt == F32:
                op(sp, lambda: nc.sync.dma_start(out=t[:], in_=cin[key]), w=[b], q=qsp)
            else:
                op(pool, lambda: G.dma_start(out=t[:], in_=cin[key]), w=[b], q=qpl)
            return t, b

        mtab, bmtab = ctile("mtab", "c_mtab", [128, 2 * 8 * 128])
        caus, bcaus = ctile("caus", "c_caus", [128, 128])
        qdtab, bqd = ctile("qdtab", "c_qd", [128, 512])
        kstab, bks = ctile("kstab", "c_ks", [128, 512])
        gctab, bgc = ctile("gctab", "c_gc", [128, 512])
        identb, bidb = ctile("identb", "c_ident", [128, 128], BF16)
        identf, bidf = ctile("identf", "c_ident", [128, 128])
        mtab4 = mtab[:].rearrange("p (a h i) -> p a h i", a=2, h=8, i=128)

        bconst = Buf("const")
        gmix = sb("gmix", [128, D], F32)
        op(sp, lambda: nc.sync.dma_start(out=gmix[:], in_=norm_mix_g.partition_broadcast(128)), w=[bconst], q=qsp)
        gffn = sb("gffn", [128, D], F32)
        bgffn = Buf()
        op(sp, lambda: nc.sync.dma_start(out=gffn[:], in_=norm_ffn_g.partition_broadcast(128)), w=[bgffn], q=qsp)
        bcol = sb("bcol", [128, 29], F32)
        bbcol = Buf("bcol")
        with nc.allow_non_contiguous_dma(reason="tiny bias column loads"):
            for c in range(4):
                op(sp, lambda c=c: nc.sync.dma_start(out=bcol[0:64, 8 + c:9 + c], in_=b_in[0:1, 64 * c:64 * c + 64].rearrange("o e -> e o")), pw=[bbcol], q=qsp)
                op(sp, lambda c=c: nc.sync.dma_start(out=bcol[64:128, 8 + c:9 + c], in_=b_in[0:1, 256 + 64 * c:256 + 64 * c + 64].rearrange("o e -> e o")), pw=[bbcol], q=qsp)
            for nm, m in (("rq", 0), ("rk", 4), ("ak", 12), ("ga", 13), ("gr", 21)):
                nchunk = WID[nm] // 128
                op(sp, lambda nm=nm, m=m, nchunk=nchunk: nc.sync.dma_start(
                    out=bcol[:, m:m + nchunk],
                    in_=b_in[0:1, SRC[nm]:SRC[nm] + WID[nm]].rearrange("o (m p) -> p (o m)", p=128)), pw=[bbcol], q=qsp)
        bhalf = sb("bhalf", [128, 16], F32)
        bbhalf = Buf()
        op(dve, lambda: V.tensor_scalar(out=bhalf[:], in0=bcol[:, 13:29], scalar1=0.5, scalar2=None, op0=ALU.mult), r=[bbcol], w=[bbhalf])
        brow_b = sb("brow_b", [1, 2176], BF16)
        bbrow = Buf()
        o = 0
        for nm in ("av", "rv", "rg"):
            op(pool, lambda o=o, nm=nm: G.dma_start(out=brow_b[0:1, o:o + WID[nm]], in_=b_in[0:1, SRC[nm]:SRC[nm] + WID[nm]]), pw=[bbrow], q=qpl)
            o += WID[nm]
        ones2 = sb("ones2", [1, 128], BF16)
        onesb = sb("onesb", [128, 128], BF16)
        mhalf = sb("mhalf", [128, 8], F32)
        bones = Buf()
        op(dve, lambda: V.memset(ones2[:], 1.0), w=[bones])
        op(dve, lambda: V.memset(onesb[:], 1.0), w=[bones])
        op(dve, lambda: V.memset(mhalf[:], -0.5), w=[bones])
        sk = sb("sk", [128, 2, 2], F32)
        esink = sb("esink", [128, 2, 2, 128], F32)
        bsk = Buf()
        for hf_ in range(2):
            for g_ in range(2):
                o_ = 4 * g_ + 2 * hf_
                op(sp, lambda hf_=hf_, g_=g_, o_=o_: nc.sync.dma_start(out=sk[64 * hf_:64 * hf_ + 64, g_, :], in_=sinks[0:1, o_:o_ + 2].partition_broadcast(64)),
                   pw=[bsk], q=qsp)
        op(act, lambda: S.activation(out=sk[:], in_=sk[:], func=AF.Exp), w=[bsk])
        op(dve, lambda: V.tensor_copy(out=esink[:].rearrange("p g c i -> p (g c) i"), in_=sk[:].rearrange("p g c -> p (g c)").unsqueeze(2).to_broadcast([128, 4, 128])),
           r=[bsk], w=[bconst])

        gfin = sb("gfin", [128, D], F32)
        bgfin = Buf()
        op(sp, lambda: nc.sync.dma_start(out=gfin[:], in_=norm_fin_g.partition_broadcast(128)), w=[bgfin], q=qsp)
        wr = sb("wr", [128, 8, 36], F32)
        bwr = Buf()
        with nc.allow_non_contiguous_dma(reason="small router weights"):
            op(sp, lambda: nc.sync.dma_start(out=wr[:, :, 0:4], in_=w_gr.rearrange("(k p) g -> p k g", p=128)), pw=[bwr], q=qsp)
            op(sp, lambda: nc.sync.dma_start(out=wr[:, :, 4:36], in_=w_er.rearrange("(k p) g -> p k g", p=128)), pw=[bwr], q=qsp)
        brt = sb("brt", [1, 36], F32)
        op(sp, lambda: nc.sync.dma_start(out=brt[:, 0:4], in_=b_gr), pw=[bwr], q=qsp)
        op(sp, lambda: nc.sync.dma_start(out=brt[:, 4:36], in_=b_er), pw=[bwr], q=qsp)
        ones_f = sb("ones_f", [1, 128], F32)
        op(dve, lambda: V.memset(ones_f[:], 1.0), pw=[bwr])
        trib, btrib = ctile("trib", "c_tri", [128, 128], BF16)
        ebase, bebase = ctile("ebase", "c_ebase", [128, NE])
        wts = sb("wts", [128, NT, 2], F32)
        bwts = Buf()
        dest_all = sb("dest_all", [128, NT, 2], I32)
        bdest = Buf()
        base_bc = sb("base_bc", [128, NE], F32)
        bbase = Buf()
        op(dve, lambda: V.memset(base_bc[:], 0.0), w=[bbase])
        bXg = Buf("Xg")
        bXg = Buf("Xg")
        bX2 = Buf("X2")
        bYg = Buf("Yg")
        breg = G.to_reg(NSLOT - 1)
        breg2 = G.to_reg(NSLOT)
        zt = sb("zt", [128, 1, D], BF16)
        bzt = Buf()
        op(dve, lambda: V.memset(zt[:], 0.0), w=[bzt])
        op(act, lambda: S.dma_start(out=Yg[NSLOT:NSLOT + 128, :], in_=zt[:, 0, :]), r=[bzt], pw=[bYg], q=kb.qact)
        bXz = Buf("Xg_zero")

        def zero_gen():
            for z_ in range(NSLOT // 128):
                op(sp, lambda z_=z_: nc.sync.dma_start(out=Xg[z_ * 128:(z_ + 1) * 128, :], in_=zt[:, 0, :]), r=[bzt], pw=[bXz], q=qsp)
                if z_ % 2 == 1:
                    yield

        zerog = zero_gen()

        def zero_tick():
            try:
                next(zerog)
            except StopIteration:
                pass

        def ensure_zero():
            for _ in zerog:
                pass
        bX2 = Buf("X2")
        bYg = Buf("Yg")

        qcast = DmaQ(kb, pool, 4, "qcast")
        kb.qcast = qcast
        NWC = 3
        wc = [kb.es.enter_context(nc.sbuf_tensor(f"wc{i}", [128, 8, 512], BF16)) for i in range(NWC)]
        bwc = [Buf() for _ in range(NWC)]
        wci = [0]
        Wb_v = Wb.rearrange("(k p) c -> p k c", p=128)
        Wru_v = Wrub.rearrange("(k p) c -> p k c", p=128)
        Wout_v = Woutb.rearrange("(k p) c -> p k c", p=128)
        win_v = w_in.rearrange("(k p) c -> p k c", p=128)
        wru_v = w_ru.rearrange("(k p) c -> p k c", p=128)
        wout_v = w_out.rearrange("(k p) c -> p k c", p=128)
        SECT = [("rq", 0), ("rk", 512), ("rv", 1024), ("rg", 2048), ("aq", 3072), ("ak", 3584), ("av", 3712), ("ga", 3840), ("gr", 4864)]

        def src_col(dcol):
            for nm, d0 in SECT:
                if d0 <= dcol < d0 + WID[nm]:
                    return SRC[nm] + (dcol - d0)

        def stage_chunk(pieces, store_dst, store_buf, n):
            i = wci[0] % NWC
            wci[0] += 1
            first = True
            for dst_fn, src in pieces:
                if first:
                    op(pool, lambda dst_fn=dst_fn, src=src: G.dma_start(out=dst_fn(wc[i]), in_=src), w=[bwc[i]], q=qcast)
                    first = False
                else:
                    op(pool, lambda dst_fn=dst_fn, src=src: G.dma_start(out=dst_fn(wc[i]), in_=src), pw=[bwc[i]], q=qcast)
            op(sp, lambda: nc.sync.dma_start(out=store_dst, in_=wc[i][:, :, 0:n]), r=[bwc[i]], pw=[store_buf], q=qsp)

        staged = set()

        def stage_gen():
            for ci in (6, 7, 0, 1, 2, 3, 4, 5, 8, 9, 10, 11):
                c0, n = WCH[ci]
                if ci == 6:
                    pcs = [((lambda t, c=c, h=h: t[:, :, 128 * c + 64 * h:128 * c + 64 * h + 64]), win_v[:, :, 256 * h + 64 * c:256 * h + 64 * c + 64])
                           for c in range(4) for h in range(2)]
                elif ci == 7:
                    pcs = [((lambda t: t[:, :, 0:128]), win_v[:, :, SRC["ak"]:SRC["ak"] + 128]),
                           ((lambda t: t[:, :, 128:256]), win_v[:, :, SRC["av"]:SRC["av"] + 128])]
                else:
                    sc_ = src_col(c0)
                    pcs = [((lambda t, n=n: t[:, :, 0:n]), win_v[:, :, sc_:sc_ + n])]
                with nc.allow_non_contiguous_dma(reason="one-time weight staging"):
                    stage_chunk(pcs, Wb_v[:, :, c0:c0 + n], BWc[ci], n)
                staged.add(ci)
                yield
            pcs = []
            for g_ in range(2):
                for hf_ in range(2):
                    for cc_ in range(2):
                        for j_ in range(2):
                            r0 = ((2 * g_ + hf_) * 2 + cc_) * 64
                            pcs.append(((lambda t, hf_=hf_, g_=g_, cc_=cc_, j_=j_: t[64 * hf_:64 * hf_ + 64, 4 * j_ + 2 * g_ + cc_, :]),
                                        w_au[r0:r0 + 64, 512 * j_:512 * j_ + 512]))
            stage_chunk(pcs, Waub, bWau, 512)
            staged.add("wau")
            yield
            for j in range(2):
                stage_chunk([((lambda t: t[:, :, 0:512]), wru_v[:, :, 512 * j:512 * j + 512])], Wru_v[:, :, 512 * j:512 * j + 512], bWru, 512)
                yield
            staged.add("wru")
            for j in range(2):
                stage_chunk([((lambda t: t[:, :, 0:512]), wout_v[:, :, 512 * j:512 * j + 512])], Wout_v[:, :, 512 * j:512 * j + 512], bWout, 512)
                yield
            staged.add("wout")

        stageg = stage_gen()
        stage_ctr = [0]

        def ensure_staged(key):
            while key not in staged:
                next(stageg)

        def stage_tick():
            stage_ctr[0] += 1
            if stage_ctr[0] % 3 == 0:
                try:
                    next(stageg)
                except StopIteration:
                    pass

        if "eager" in taps:
            for _ in stageg:
                pass
            kb.barrier()
        else:
            ensure_staged(7)
        es_mix = ExitStack()
        kb.cur = es_mix
        x_sb = sb("x_sb", [128, 2, D], F32)
        bx = [Buf(f"x{t}") for t in range(2)]
        ssq = sb("ssq", [128, 8], F32)
        rstd = sb("rstd", [128, 8], F32)
        bst = [Buf() for _ in range(4)]
        xn = [sb(f"xn{i}", [128, D], BF16) for i in range(2)]
        bxn = [Buf(), Buf()]
        xnT = sb("xnT", [128, 8, 512], BF16)
        bxnT = [Buf() for _ in range(4)]
        QT = sb("QT", [128, 4, 512], BF16)
        bQT = Buf()
        KT = sb("KT", [128, SEQ], BF16)
        bKT = [Buf() for _ in range(NBLK)]
        VA = sb("VA", [128, NBLK, 128], BF16)
        bVA = [Buf() for _ in range(NBLK)]
        qdT = sb("qdT", [128, 4, 512], BF16)
        bqdT = Buf()
        ksT = sb("ksT", [128, 4, 512], BF16)
        bksT = Buf()
        ta = sb("ta", [128, 8, 512], BF16)
        tr_ = sb("tr", [128, 8, 512], BF16)
        bta = [Buf() for _ in range(8)]
        btr = [Buf() for _ in range(8)]
        Vr = [sb(f"Vr{t}", [128, D], BF16) for t in range(4)]
        bVr = [Buf() for _ in range(4)]
        sg = [sb(f"sg{t}", [128, D], BF16) for t in range(4)]
        bsg = [Buf() for _ in range(4)]
        tg = sb("tg", [128, 512], F32)
        btg = Buf()
        eS = [sb(f"eS{i}", [128, 512], F32) for i in range(2)]
        beS = [Buf(), Buf()]
        PT = [sb(f"PT{i}", [128, 512], BF16) for i in range(4)]
        bPT = [Buf() for _ in range(4)]
        rden = sb("rden", [128, 256], F32)
        brden = Buf()
        attnT = sb("attnT", [128, 2, 2, 512], BF16)
        battn = [Buf() for _ in range(4)]
        scT = sb("scT", [128, 2, 4, 128], BF16)
        bscT = Buf()
        kstok = sb("kstok", [128, 4, 128], BF16)
        bkstok = Buf()
        state = sb("state", [128, 512], F32)
        stmp = sb("stmp", [128, 512], F32)
        state_bf = [sb(f"state_bf{i}", [128, 512], BF16) for i in range(2)]
        bstate = Buf()
        bstmp = Buf()
        bstbf = [Buf(), Buf()]
        st8_2 = [sb(f"st8_{i}", [128, 8, 8], F32) for i in range(2)]
        bst8_2 = [Buf(), Buf()]
        x2 = [sb(f"x2_{i}", [128, D], F32) for i in range(2)]
        bx2 = [Buf(), Buf()]
        ysq, bysq = x2[0], bx2[0]
        yn, byn = x2[1], bx2[1]
        retg = sb("retg", [128, D], BF16)
        bretg = Buf()
        junk, bjunk = retg, bretg
        retgT = sb("retgT", [128, 8, 512], BF16)
        bretgT = [Buf() for _ in range(4)]
        mixT = sb("mixT", [128, 8, 512], BF16)
        bmix = [Buf() for _ in range(8)]
        m1, m2, bm1, bm2 = eS[0], eS[1], beS[0], beS[1]
        h2f = sb("h2f", [128, D], F32)
        bh2f = Buf()
        h2b = [sb(f"h2b{i}", [128, D], BF16) for i in range(2)]
        bh2b = [Buf(), Buf()]
        h2T = sb("h2T", [128, 8, 128], F32)
        bh2T = Buf()
        R = sb("R", [128, 384], F32)
        bR = Buf()
        selb = sb("selb", [128, NE], BF16)
        bselb = Buf()
        rs = sb("rs", [128, 2], F32)
        brs = Buf()

        if "mem" in taps:
            print("SBUF bytes remaining/partition after mixer alloc:", nc.sbuf_bytes_remaining)
        def bview(bank_t, a, b_):
            return bank_t[:].rearrange("p (a b) -> p a b", a=a, b=b_)

        def load_w(src_ap, rb, n=512):
            i = wci[0] % NWC
            wci[0] += 1
            op(sp, lambda: nc.sync.dma_start(out=wc[i][:, :, 0:n], in_=src_ap), r=[rb], w=[bwc[i]], q=qsp)
            return wc[i], bwc[i]


        def route_a(tt, xi):
            lg, gmax, ngmax, gsum, gprob = R[:, 0:36], R[:, 36:37], R[:, 37:38], R[:, 38:39], R[:, 39:40]
            goh, t1, jk4 = R[:, 40:44], R[:, 44:48], R[:, 48:52]
            em, top8, sel1, sel2 = R[:, 64:96], R[:, 96:104], R[:, 104:136], R[:, 136:168]
            dlt, e2, den, rr = R[:, 168:169], R[:, 169:170], R[:, 170:171], R[:, 171:172]
            posf, okm, nbig, tmp, tmp1, d12 = R[:, 176:208], R[:, 208:240], R[:, 240:272], R[:, 272:304], R[:, 304:336], R[:, 336:338]
            hb = tt % 2
            op(act, lambda: S.activation(out=junk[:], in_=x2[xi][:], func=AF.Square, accum_out=rs[:, 0:1]), r=[bx2[xi]], w=[bjunk, brs])
            op(dve, lambda: V.tensor_scalar(out=rs[:, 0:1], in0=rs[:, 0:1], scalar1=1.0 / D, scalar2=EPS, op0=ALU.mult, op1=ALU.add), w=[brs])
            op(pool, lambda: G.tensor_tensor(out=rs[:, 1:2], in0=rs[:, 0:1], in1=mhalf[:, 0:1], op=ALU.pow), r=[bones], w=[brs])
            op(dve, lambda: V.scalar_tensor_tensor(out=h2f[:], in0=x2[xi][:], scalar=rs[:, 1:2], in1=gffn[:], op0=ALU.mult, op1=ALU.mult),
               r=[bx2[xi], brs, bgffn], w=[bh2f])
            op(pool, lambda: G.tensor_copy(out=h2b[hb][:], in_=h2f[:]), r=[bh2f], w=[bh2b[hb]])

        def route_b(tt, xi):
            lg, gmax, ngmax, gsum, gprob = R[:, 0:36], R[:, 36:37], R[:, 37:38], R[:, 38:39], R[:, 39:40]
            goh, t1, jk4 = R[:, 40:44], R[:, 44:48], R[:, 48:52]
            em, top8, sel1, sel2 = R[:, 64:96], R[:, 96:104], R[:, 104:136], R[:, 136:168]
            dlt, e2, den, rr = R[:, 168:169], R[:, 169:170], R[:, 170:171], R[:, 171:172]
            posf, okm, nbig, tmp, tmp1, d12 = R[:, 176:208], R[:, 208:240], R[:, 240:272], R[:, 272:304], R[:, 304:336], R[:, 336:338]
            hb = tt % 2
            yield
            for j in range(2):
                bt, bb = kb.bank("B")

                def trf(bt=bt, j=j):
                    ins = None
                    for k in range(4):
                        kk = 4 * j + k
                        ins = P_.transpose(out=bt[:, k * 128:(k + 1) * 128], in_=h2f[:, kk * 128:(kk + 1) * 128], identity=identf[:])
                    return ins
                op(pe, trf, r=[bh2f, bidf], w=[bb])
                op(act, lambda bt=bt, j=j: S.copy(out=h2T[:, 4 * j:4 * j + 4, :], in_=bview(bt, 4, 128)), r=[bb], pw=[bh2T])
            yield
            bt, bb = kb.bank("B")

            def mmr_(bt=bt):
                P_.matmul(bt[:, 0:36], lhsT=ones_f[:], rhs=brt[:], start=True, stop=False)
                ins = None
                for k in range(8):
                    ins = P_.matmul(bt[:, 0:36], lhsT=h2T[:, k, :], rhs=wr[:, k, :], start=False, stop=(k == 7))
                return ins
            op(pe, mmr_, r=[bh2T, bwr], w=[bb])
            op(dve, lambda: V.tensor_copy(out=lg, in_=bt[:, 0:36]), r=[bb], w=[bR])

            def c(fn, E=dve, **kw):
                op(E, fn, w=[bR], **kw)
            c(lambda: V.tensor_reduce(out=gmax, in_=R[:, 0:4], axis=AX.X, op=ALU.max))
            c(lambda: V.tensor_scalar(out=goh, in0=R[:, 0:4], scalar1=gmax, scalar2=None, op0=ALU.is_equal))
            c(lambda: V.tensor_scalar(out=t1, in0=goh, scalar1=-1.0, scalar2=1e30, op0=ALU.add, op1=ALU.mult))
            c(lambda: V.tensor_tensor(out=em.rearrange("p (g e) -> p g e", g=4), in0=R[:, 4:36].rearrange("p (g e) -> p g e", g=4),
                                      in1=goh.unsqueeze(2).to_broadcast([128, 4, 8]), op=ALU.mult))
            c(lambda: V.tensor_tensor(out=em.rearrange("p (g e) -> p g e", g=4), in0=em.rearrange("p (g e) -> p g e", g=4),
                                      in1=t1.unsqueeze(2).to_broadcast([128, 4, 8]), op=ALU.add))
            yield
            c(lambda: V.max(out=top8, in_=em))
            c(lambda: V.tensor_scalar(out=sel1, in0=em, scalar1=top8[:, 0:1], scalar2=None, op0=ALU.is_equal))
            c(lambda: V.tensor_scalar(out=sel2, in0=em, scalar1=top8[:, 1:2], scalar2=None, op0=ALU.is_equal))
            op(dve, lambda: V.tensor_tensor(out=selb[:], in0=sel1, in1=sel2, op=ALU.add), r=[bR], w=[bselb])
            yield
            btp, bbp = kb.bank("B")

            def mmp(btp=btp):
                P_.matmul(btp[:, 0:32], lhsT=trib[:], rhs=selb[:], start=True, stop=True)
                return P_.matmul(btp[:, 32:64], lhsT=onesb[:], rhs=selb[:], start=True, stop=True)
            op(pe, mmp, r=[bselb, btrib, bones], w=[bbp])
            c(lambda: V.tensor_scalar(out=ngmax, in0=gmax, scalar1=-1.0, scalar2=None, op0=ALU.mult))
            c(lambda: S.activation(out=jk4, in_=R[:, 0:4], func=AF.Exp, bias=ngmax, scale=1.0, accum_out=gsum), E=act)
            c(lambda: V.tensor_tensor(out=dlt, in0=top8[:, 1:2], in1=top8[:, 0:1], op=ALU.subtract))
            c(lambda: S.activation(out=e2, in_=dlt, func=AF.Exp), E=act)
            c(lambda: V.reciprocal(out=gprob, in_=gsum))
            c(lambda: V.tensor_scalar(out=den, in0=e2, scalar1=1.0, scalar2=None, op0=ALU.add))
            c(lambda: V.reciprocal(out=rr, in_=den))
            op(dve, lambda: V.scalar_tensor_tensor(out=wts[:, tt, 0:1], in0=gprob, scalar=0.5, in1=rr, op0=ALU.mult, op1=ALU.mult), r=[bR], pw=[bwts])
            op(dve, lambda: V.tensor_tensor(out=wts[:, tt, 1:2], in0=wts[:, tt, 0:1], in1=e2, op=ALU.mult), r=[bR], w=[bwts])
            yield
            op(dve, lambda: V.tensor_tensor(out=posf, in0=btp[:, 0:32], in1=base_bc[:], op=ALU.add), r=[bbp, bbase], w=[bR])
            op(dve, lambda: V.tensor_tensor(out=base_bc[:], in0=btp[:, 32:64], in1=base_bc[:], op=ALU.add), r=[bbp], w=[bbase])
            yield
            c(lambda: V.tensor_scalar(out=okm, in0=posf, scalar1=float(CAP), scalar2=None, op0=ALU.is_lt))
            c(lambda: V.tensor_scalar(out=nbig, in0=okm, scalar1=-1.0, scalar2=-float(OOB), op0=ALU.add, op1=ALU.mult))
            c(lambda: V.tensor_tensor(out=tmp, in0=posf, in1=ebase[:], op=ALU.add), r=[bebase])
            c(lambda: V.tensor_tensor(out=tmp, in0=tmp, in1=okm, op=ALU.mult))
            c(lambda: V.tensor_tensor(out=tmp, in0=tmp, in1=nbig, op=ALU.add))
            c(lambda: V.tensor_tensor(out=tmp1, in0=tmp, in1=sel1, op=ALU.mult))
            c(lambda: V.tensor_reduce(out=d12[:, 0:1], in_=tmp1, axis=AX.X, op=ALU.add))
            c(lambda: V.tensor_tensor(out=tmp1, in0=tmp, in1=sel2, op=ALU.mult))
            c(lambda: V.tensor_reduce(out=d12[:, 1:2], in_=tmp1, axis=AX.X, op=ALU.add))
            op(dve, lambda: V.tensor_copy(out=dest_all[:, tt, :], in_=d12), r=[bR], pw=[bdest])
            for k in range(2):
                op(pool, lambda k=k: G.indirect_dma_start(out=Xg[:, :], out_offset=bass.IndirectOffsetOnAxis(ap=dest_all[:, tt, k:k + 1], axis=0),
                                                          in_=h2b[hb][:], in_offset=None, bounds_check=breg, oob_is_err=False),
                   r=[bh2b[hb], bdest, bXz], pw=[bXg], q=qpl)
            if "route" in taps and tt == 0:
                tapo["R"] = nc.dram_tensor("tap_R", [128, 384], F32, kind="ExternalOutput").ap()
                op(sp, lambda: nc.sync.dma_start(out=tapo["R"], in_=R[:]), r=[bR], q=qsp)

        def gen_norm(s, q):
            tok0 = s * SEQ + q * 512
            for t in range(4):
                xb_ = t % 2
                op(sp, lambda t=t, xb_=xb_: nc.sync.dma_start(out=x_sb[:, xb_, :], in_=x[tok0 + t * 128: tok0 + (t + 1) * 128, :]), w=[bx[xb_]], q=qsp)
                op(act, lambda t=t, xb_=xb_: S.activation(out=junk[:], in_=x_sb[:, xb_, :], func=AF.Square, accum_out=ssq[:, t:t + 1]),
                   r=[bx[xb_]], w=[bjunk, bst[t]])
                op(dve, lambda t=t: V.tensor_scalar(out=ssq[:, t:t + 1], in0=ssq[:, t:t + 1], scalar1=1.0 / D, scalar2=EPS, op0=ALU.mult, op1=ALU.add),
                   w=[bst[t]])
                op(pool, lambda t=t: G.tensor_tensor(out=rstd[:, t:t + 1], in0=ssq[:, t:t + 1], in1=mhalf[:, 0:1], op=ALU.pow),
                   r=[bones], w=[bst[t]])
                yield
                xi = t % 2
                op(dve, lambda t=t, xi=xi, xb_=xb_: V.scalar_tensor_tensor(out=xn[xi][:], in0=x_sb[:, xb_, :], scalar=rstd[:, t:t + 1], in1=gmix[:],
                                                                          op0=ALU.mult, op1=ALU.mult),
                   r=[bx[xb_], bst[t], bconst], w=[bxn[xi]])
                yield
                bt, bb = kb.bank("B")
                btv = bt[:].bitcast(BF16).rearrange("p (k c) -> p k c", k=8)

                def tr8(xi=xi, btv=btv):
                    ins = None
                    for k in range(8):
                        ins = P_.transpose(out=btv[:, k, :], in_=xn[xi][:, k * 128:(k + 1) * 128], identity=identb[:])
                    return ins
                op(pe, tr8, r=[bxn[xi], bidb], w=[bb])
                op(act, lambda t=t, btv=btv: S.copy(out=xnT[:, :, t * 128:(t + 1) * 128], in_=btv), r=[bb], w=[bxnT[t]])
                yield

        def fm_chunk(q, wt, bw, wcol, m):
            bt, bb = kb.bank("A")

            def mm():
                ins = None
                for k in range(8):
                    ins = P_.matmul(bt[:], lhsT=wt[:, k, wcol:wcol + 128], rhs=xnT[:, k, :], start=(k == 0), stop=(k == 7))
                return ins
            op(pe, mm, r=[bw] + bxnT, w=[bb])
            bias = bcol[:, m:m + 1]
            if m < 4:
                c = m
                op(dve, lambda: V.scalar_tensor_tensor(out=qdT[:, c, :].rearrange("p (t i) -> p t i", t=4), in0=bview(bt, 4, 128), scalar=bias,
                                                       in1=qdtab[:, c * 128:(c + 1) * 128].unsqueeze(1).to_broadcast([128, 4, 128]),
                                                       op0=ALU.add, op1=ALU.mult), r=[bb, bbcol, bqd], pw=[bqdT])
            elif m < 8:
                c = m - 4
                op(dve, lambda: V.scalar_tensor_tensor(out=ksT[:, c, :].rearrange("p (t i) -> p t i", t=4), in0=bview(bt, 4, 128), scalar=bias,
                                                       in1=kstab[:, c * 128:(c + 1) * 128].unsqueeze(1).to_broadcast([128, 4, 128]),
                                                       op0=ALU.add, op1=ALU.mult), r=[bb, bbcol, bks], pw=[bksT])
            elif m < 12:
                op(dve, lambda: V.tensor_scalar(out=QT[:, m - 8, :], in0=bt[:], scalar1=bias, scalar2=None, op0=ALU.add), r=[bb, bbcol], pw=[bQT])
            elif m == 12:
                blk0 = q * 4
                op(dve, lambda: V.tensor_scalar(out=KT[:, blk0 * 128:blk0 * 128 + 512], in0=bt[:], scalar1=bias, scalar2=None, op0=ALU.add),
                   r=[bb, bbcol], w=bKT[blk0:blk0 + 4])
            elif m < 21:
                k_ = m - 13
                op(act, lambda: S.activation(out=ta[:, k_, :], in_=bt[:], func=AF.Tanh, bias=bhalf[:, m - 13:m - 12], scale=0.5), r=[bb, bbhalf], w=[bta[k_]])
            else:
                k_ = m - 21
                op(act, lambda: S.activation(out=tr_[:, k_, :], in_=bt[:], func=AF.Tanh, bias=bhalf[:, m - 13:m - 12], scale=0.5), r=[bb, bbhalf], w=[btr[k_]])

        FMCH = {0: (0, 0), 1: (1, 4), 6: (6, 8), 7: (7, 12), 8: (8, 13), 9: (9, 17), 10: (10, 21), 11: (11, 25)}

        def load_wch(ci):
            ensure_staged(ci)
            c0, n = WCH[ci]
            return load_w(Wb_v[:, :, c0:c0 + n], BWc[ci], n)

        def gen_fm(q, chunks):
            for ci in chunks:
                wt, bw = load_wch(ci)
                m = FMCH[ci][1]
                for cc in range(1 if ci == 7 else 4):
                    fm_chunk(q, wt, bw, cc * 128, m + cc)
                    yield
                if ci == 7:
                    blk0 = q * 4
                    for t in range(4):
                        bt, bb = kb.bank("A")

                        def mmav(t=t, bt=bt, wt=wt):
                            P_.matmul(bt[:, 0:128], lhsT=ones2[:], rhs=brow_b[:, 0:128], start=True, stop=False)
                            ins = None
                            for k in range(8):
                                ins = P_.matmul(bt[:, 0:128], lhsT=xnT[:, k, t * 128:(t + 1) * 128], rhs=wt[:, k, 128:256], start=False, stop=(k == 7))
                            return ins
                        op(pe, mmav, r=[bw, bxnT[t], bbrow, bones], w=[bb])
                        op(dve, lambda t=t, bt=bt: V.tensor_copy(out=VA[:, blk0 + t, :], in_=bt[:, 0:128]), r=[bb], w=[bVA[blk0 + t]])
                    yield

        def gen_tm(q, js=(0, 1, 2, 3)):
            for j in js:
                wt, bw = load_wch(2 + j)
                for t in range(4):
                    bt, bb = kb.bank("A")
                    bo = 128 + j * 512

                    def mmtm(t=t, bt=bt, wt=wt, bo=bo):
                        P_.matmul(bt[:], lhsT=ones2[:], rhs=brow_b[:, bo:bo + 512], start=True, stop=False)
                        ins = None
                        for k in range(8):
                            ins = P_.matmul(bt[:], lhsT=xnT[:, k, t * 128:(t + 1) * 128], rhs=wt[:, k, :], start=False, stop=(k == 7))
                        return ins
                    op(pe, mmtm, r=[bw, bxnT[t], bbrow, bones], w=[bb])
                    if j < 2:
                        op(act, lambda t=t, bt=bt, j=j: S.copy(out=Vr[t][:, j * 512:(j + 1) * 512], in_=bt[:]), r=[bb], pw=[bVr[t]])
                    else:
                        jj = j - 2
                        op(act, lambda bt=bt: S.activation(out=tg[:], in_=bt[:], func=AF.Tanh, scale=0.5), r=[bb], w=[btg])
                        op(dve, lambda t=t, bt=bt, jj=jj: V.scalar_tensor_tensor(out=sg[t][:, jj * 512:(jj + 1) * 512], in0=tg[:], scalar=1.0, in1=bt[:],
                                                                               op0=ALU.add, op1=ALU.mult), r=[bb, btg], pw=[bsg[t]])
                    yield

        def gen_att(q):
            def step1(t, g):
                blk = q * 4 + t
                pts = []
                for pc in ((0, 1) if blk > 0 else (1,)):
                    kblk = blk - 1 + pc
                    bt, bb = kb.bank("B")
                    op(pe, lambda bt=bt, kblk=kblk: P_.matmul(
                        bview(bt, 4, 128), lhsT=KT[64 * g:64 * g + 64, kblk * 128:(kblk + 1) * 128],
                        rhs=QT[64 * g:64 * g + 64, :, t * 128:(t + 1) * 128], start=True, stop=True),
                       r=[bKT[kblk], bQT], w=[bb])
                    ei = pc
                    op(act, lambda bt=bt, ei=ei: S.activation(out=eS[ei][:], in_=bt[:], func=AF.Exp, scale=0.125), r=[bb], w=[beS[ei]])
                    pi = 2 * g + pc
                    op(dve, lambda ei=ei, pi=pi, pc=pc: V.tensor_tensor(
                        out=PT[pi][:].rearrange("p (c i) -> p c i", c=4), in0=eS[ei][:].rearrange("p (c i) -> p c i", c=4),
                        in1=mtab4[:, pc, 4 * g:4 * g + 4, :], op=ALU.mult), r=[beS[ei], bmtab], w=[bPT[pi]])
                    pts.append((pi, kblk))
                return pts

            def step2(t, g, pts):
                btn, bbn = kb.bank("B")
                btd, bbd = kb.bank("B")

                def pv():
                    ins = None
                    last = len(pts) - 1
                    for hf in range(2):
                        for n_, (pi, kblk) in enumerate(pts):
                            P_.matmul(btn[64 * hf:64 * hf + 64, 0:256], lhsT=VA[:, kblk, 64 * g:64 * g + 64], rhs=PT[pi][:, 256 * hf:256 * hf + 256],
                                      start=(n_ == 0), stop=(n_ == last))
                    for hf in range(2):
                        for n_, (pi, kblk) in enumerate(pts):
                            ins = P_.matmul(btd[64 * hf:64 * hf + 64, 0:256], lhsT=onesb[:, 0:64], rhs=PT[pi][:, 256 * hf:256 * hf + 256],
                                            start=(n_ == 0), stop=(n_ == last))
                    return ins
                op(pe, pv, r=[bPT[pi] for pi, _ in pts] + [bVA[kb_] for _, kb_ in pts] + [bones], w=[bbn, bbd])
                op(dve, lambda: V.tensor_tensor(out=rden[:].rearrange("p (c i) -> p c i", c=2), in0=btd[:, 0:256].rearrange("p (c i) -> p c i", c=2),
                                                in1=esink[:, g, :, :], op=ALU.add), r=[bbd, bconst], w=[brden])
                op(dve, lambda: V.reciprocal(out=rden[:], in_=rden[:]), w=[brden])
                op(dve, lambda: V.tensor_tensor(out=attnT[:, g, :, t * 128:(t + 1) * 128], in0=btn[:, 0:256].rearrange("p (c i) -> p c i", c=2),
                                                in1=rden[:].rearrange("p (c i) -> p c i", c=2), op=ALU.mult),
                   r=[bbn, brden], pw=[battn[t]])

            pend = None
            for t in range(4):
                for g in range(2):
                    pts = step1(t, g)
                    yield
                    if pend is not None:
                        step2(*pend)
                        yield
                    pend = (t, g, pts)
            step2(*pend)
            yield

        def gen_ret(q):
            ybanks = {}

            def sa(t):
                tc0, tc1 = t * 128, (t + 1) * 128
                for p in range(2):
                    bt, bb = kb.bank("B")

                    def mmsc(bt=bt, p=p):
                        ins = None
                        for c in range(4):
                            ins = P_.matmul(bt[:, c * 128:(c + 1) * 128], lhsT=ksT[64 * p:64 * p + 64, c, tc0:tc1], rhs=qdT[64 * p:64 * p + 64, c, tc0:tc1],
                                            start=True, stop=True)
                        return ins
                    op(pe, mmsc, r=[bksT, bqdT], w=[bb])
                    op(dve, lambda bt=bt, p=p: V.tensor_tensor(out=scT[:, p, :, :], in0=bview(bt, 4, 128),
                                                              in1=caus[:].unsqueeze(1).to_broadcast([128, 4, 128]), op=ALU.mult),
                       r=[bb, bcaus], pw=[bscT])
                bt, bb = kb.bank("B")
                btv = bt[:].bitcast(BF16).rearrange("p (k c) -> p k c", k=8)

                def trk():
                    ins = None
                    for c in range(4):
                        ins = P_.transpose(out=btv[:, c, :], in_=ksT[:, c, tc0:tc1], identity=identb[:])
                    return ins
                op(pe, trk, r=[bksT, bidb], w=[bb])
                op(act, lambda: S.copy(out=kstok[:], in_=btv[:, 0:4, :]), r=[bb], w=[bkstok])

            def sbm(t):
                blk = q * 4 + t
                tc0, tc1 = t * 128, (t + 1) * 128
                if blk == 0:
                    op(dve, lambda: V.memset(state[:], 0.0), w=[bstate])
                    op(dve, lambda: V.memset(state_bf[0][:], 0.0), w=[bstbf[0]])
                sbi = blk % 2
                yb = []
                for p in range(2):
                    bt, bb = kb.bank("Y")

                    def mmy(bt=bt, p=p):
                        ins = None
                        for c in range(4):
                            h = 2 * c + p
                            P_.matmul(bt[:, c * 128:(c + 1) * 128], lhsT=scT[:, p, c, :], rhs=Vr[t][:, h * 128:(h + 1) * 128], start=True, stop=False)
                            ins = P_.matmul(bt[:, c * 128:(c + 1) * 128], lhsT=qdT[64 * p:64 * p + 64, c, tc0:tc1],
                                            rhs=state_bf[sbi][64 * p:64 * p + 64, c * 128:(c + 1) * 128], start=False, stop=True)
                        return ins
                    op(pe, mmy, r=[bscT, bVr[t], bqdT, bstbf[sbi]], w=[bb])
                    yb.append((bt, bb))
                ybanks[t] = yb
                btk, bbk = kb.bank("B")

                def mmkv():
                    ins = None
                    for c in range(4):
                        for p in range(2):
                            h = 2 * c + p
                            ins = P_.matmul(btk[64 * p:64 * p + 64, c * 128:(c + 1) * 128], lhsT=kstok[:, c, 64 * p:64 * p + 64],
                                            rhs=Vr[t][:, h * 128:(h + 1) * 128], start=True, stop=True)
                    return ins
                op(pe, mmkv, r=[bkstok, bVr[t]], w=[bbk])
                op(dve, lambda: V.tensor_tensor(out=stmp[:], in0=btk[:], in1=state[:], op=ALU.add), r=[bbk, bstate], w=[bstmp])
                op(dve, lambda: V.tensor_tensor(out=state[:], in0=stmp[:], in1=gctab[:], op=ALU.mult), r=[bstmp, bgc], w=[bstate])
                op(pool, lambda: G.tensor_copy(out=state_bf[1 - sbi][:], in_=state[:]), r=[bstate], w=[bstbf[1 - sbi]])

            def sc(t):
                st8, bst8 = st8_2[t % 2], bst8_2[t % 2]
                for p, (bt, bb) in enumerate(ybanks[t]):
                    op(dve, lambda bt=bt, p=p: V.tensor_reduce(out=st8[:, 0, 4 * p:4 * p + 4], in_=bview(bt, 4, 128), axis=AX.X, op=ALU.add), r=[bb], pw=[bst8])
                    op(act, lambda bt=bt, p=p: S.activation(out=ysq[:, 512 * p:512 * (p + 1)], in_=bt[:], func=AF.Square), r=[bb], pw=[bysq])
                op(dve, lambda: V.tensor_reduce(out=st8[:, 1, :], in_=ysq[:].rearrange("p (h e) -> p h e", h=8), axis=AX.X, op=ALU.add), r=[bysq], pw=[bst8])
                op(dve, lambda: V.scalar_tensor_tensor(out=st8[:, 3, :], in0=st8[:, 0, :], scalar=1.0 / (128.0 * 128.0), in1=st8[:, 0, :], op0=ALU.mult, op1=ALU.mult), w=[bst8])
                op(dve, lambda: V.scalar_tensor_tensor(out=st8[:, 4, :], in0=st8[:, 1, :], scalar=1.0 / 128, in1=st8[:, 3, :], op0=ALU.mult, op1=ALU.subtract), w=[bst8])
                op(pool, lambda: G.tensor_scalar(out=st8[:, 4, :], in0=st8[:, 4, :], scalar1=4.0, scalar2=4.0 * EPS, op0=ALU.mult, op1=ALU.add), w=[bst8])
                op(pool, lambda: G.tensor_tensor(out=st8[:, 6, :], in0=st8[:, 4, :], in1=mhalf[:], op=ALU.pow), r=[bones], w=[bst8])
                op(dve, lambda: V.scalar_tensor_tensor(out=st8[:, 7, :], in0=st8[:, 0, :], scalar=-1.0 / 128, in1=st8[:, 6, :], op0=ALU.mult, op1=ALU.mult), w=[bst8])

            def sd(t):
                st8, bst8 = st8_2[t % 2], bst8_2[t % 2]
                for p, (bt, bb) in enumerate(ybanks[t]):
                    def nrm(bt=bt, p=p):
                        ins = None
                        for c in range(4):
                            h = 2 * c + p
                            si = 4 * p + c
                            ins = S.activation(out=yn[:, h * 128:(h + 1) * 128], in_=bt[:, c * 128:(c + 1) * 128], func=AF.Identity,
                                               scale=st8[:, 6, si:si + 1], bias=st8[:, 7, si:si + 1])
                        return ins
                    op(act, nrm, r=[bb, bst8], pw=[byn])
                op(dve, lambda: V.tensor_tensor(out=retg[:], in0=yn[:], in1=sg[t][:], op=ALU.mult), r=[byn, bsg[t]], w=[bretg])
                yield
                bt, bb = kb.bank("B")
                btv = bt[:].bitcast(BF16).rearrange("p (k c) -> p k c", k=8)

                def trr():
                    ins = None
                    for k in range(8):
                        ins = P_.transpose(out=btv[:, k, :], in_=retg[:, k * 128:(k + 1) * 128], identity=identb[:])
                    return ins
                op(pe, trr, r=[bretg, bidb], w=[bb])
                op(act, lambda: S.copy(out=retgT[:, :, t * 128:(t + 1) * 128], in_=btv), r=[bb], w=[bretgT[t]])

            sa(0); yield
            sbm(0); yield
            sc(0); yield
            for t in range(1, 4):
                sa(t); yield
                sbm(t); yield
                sc(t); yield
                yield from sd(t - 1); yield
            yield from sd(3); yield

        def gen_up(q):
            ensure_staged("wau")
            ensure_staged("wru")
            wa, bwa = load_w(Waub, bWau)
            wr_ = [load_w(Wru_v[:, :, j * 512:(j + 1) * 512], bWru) for j in range(2)]
            for dm in range(8):
                bta_, bba = kb.bank("A")
                btr2, bbr = kb.bank("A")
                j, dc = dm // 4, (dm % 4) * 128

                def mma(bta_=bta_, j=j, dc=dc):
                    ins = None
                    for kk in range(4):
                        ins = P_.matmul(bta_[:], lhsT=wa[:, 4 * j + kk, dc:dc + 128], rhs=attnT[:, kk // 2, kk % 2, :], start=(kk == 0), stop=(kk == 3))
                    return ins

                def mmr(btr2=btr2, j=j, dc=dc):
                    ins = None
                    for k in range(8):
                        ins = P_.matmul(btr2[:], lhsT=wr_[j][0][:, k, dc:dc + 128], rhs=retgT[:, k, :], start=(k == 0), stop=(k == 7))
                    return ins
                op(pe, mma, r=battn + [bwa], w=[bba])
                op(pe, mmr, r=bretgT + [wr_[j][1]], w=[bbr])
                op(dve, lambda bta_=bta_, dm=dm: V.scalar_tensor_tensor(out=m1[:], in0=ta[:, dm, :], scalar=1.0, in1=bta_[:], op0=ALU.add, op1=ALU.mult),
                   r=[bba, bta[dm]], w=[bm1])
                op(dve, lambda btr2=btr2, dm=dm: V.scalar_tensor_tensor(out=m2[:], in0=tr_[:, dm, :], scalar=1.0, in1=btr2[:], op0=ALU.add, op1=ALU.mult),
                   r=[bbr, btr[dm]], w=[bm2])
                op(pool, lambda dm=dm: G.tensor_tensor(out=mixT[:, dm, :], in0=m1[:], in1=m2[:], op=ALU.add), r=[bm1, bm2], w=[bmix[dm]])
                yield

        def gen_out(s, q, routes):
            tok0 = s * SEQ + q * 512
            ensure_staged("wout")
            wo = [load_w(Wout_v[:, :, j * 512:(j + 1) * 512], bWout) for j in range(2)]
            for t in range(4):
                xi = t % 2
                tokr = slice(tok0 + t * 128, tok0 + (t + 1) * 128)
                op(sp, lambda xi=xi, tokr=tokr: nc.sync.dma_start(out=x2[xi][:], in_=x[tokr, :]), w=[bx2[xi]], q=qsp)
                for hf in range(2):
                    bt, bb = kb.bank("A")

                    def mmo(bt=bt, t=t, hf=hf):
                        ins = None
                        for k in range(8):
                            ins = P_.matmul(bt[:], lhsT=mixT[:, k, t * 128:(t + 1) * 128], rhs=wo[hf][0][:, k, :], start=(k == 0), stop=(k == 7))
                        return ins
                    op(pe, mmo, r=bmix + [wo[hf][1]], w=[bb])
                    op(dve, lambda bt=bt, hf=hf, xi=xi: V.scalar_tensor_tensor(out=x2[xi][:, hf * 512:(hf + 1) * 512], in0=bt[:], scalar=0.5,
                                                                                in1=x2[xi][:, hf * 512:(hf + 1) * 512], op0=ALU.mult, op1=ALU.add),
                       r=[bb], w=[bx2[xi]])
                    yield
                op(sp, lambda xi=xi, tokr=tokr: nc.sync.dma_start(out=X2[tokr, :], in_=x2[xi][:]), r=[bx2[xi]], pw=[bX2], q=qsp)
                if "x2" in taps:
                    if "x2" not in tapo:
                        tapo["x2"] = nc.dram_tensor("tap_x2", [T, D], F32, kind="ExternalOutput").ap()
                    op(sp, lambda xi=xi, tokr=tokr: nc.sync.dma_start(out=tapo["x2"][tokr, :], in_=x2[xi][:]), r=[bx2[xi]], q=qsp)
                if stop_after != "mixer":
                    drain_bg()
                    ensure_zero()
                    route_a((tok0 // 128) + t, xi)
                    bgs.append(route_b((tok0 // 128) + t, xi))

        bgs = []

        def step_all(lst):
            for b in list(lst):
                try:
                    next(b)
                except StopIteration:
                    lst.remove(b)

        def drain_bg():
            while bgs:
                step_all(bgs)

        def run_a(a, bs):
            for _ in a:
                step_all(bs)
                step_all(bgs)
                stage_tick()
                zero_tick()

        def drain(bs):
            while bs:
                step_all(bs)
                step_all(bgs)

        def chain(*gens):
            for g_ in gens:
                yield from g_

        groups = [(s_, q_) for s_ in range(nseq) for q_ in range(4)]
        if "nogroups" in taps:
            groups = []
        elif "onegroup" in taps:
            groups = groups[:1]
        for _ in (gen_norm(*groups[0]) if groups else ()):
            pass
        for gi, (s_, q_) in enumerate(groups):
            bs = []
            if "e1" in taps:
                run_a(chain(gen_fm(q_, [0, 1]), gen_tm(q_), gen_fm(q_, [6, 7, 8, 9, 10, 11])), bs)
                bs.append(gen_ret(q_))
                bs.append(gen_att(q_))
                drain(bs)
            elif "e6" in taps:
                run_a(chain(gen_fm(q_, [6, 7]), gen_fm(q_, [0, 1]), gen_tm(q_)), bs)
                bs.append(gen_att(q_))
                run_a(gen_fm(q_, [8, 9, 10, 11]), bs)
                drain(bs)
                bs.append(gen_ret(q_))
                drain(bs)
            elif "e7" in taps:
                run_a(chain(gen_fm(q_, [0, 1]), gen_tm(q_)), bs)
                bs.append(gen_ret(q_))
                run_a(gen_fm(q_, [6, 7]), bs)
                drain(bs)
                bs.append(gen_att(q_))
                drain(bs)
                run_a(gen_fm(q_, [8, 9, 10, 11]), bs)
            elif "e3" in taps:
                run_a(chain(gen_fm(q_, [0, 1]), gen_tm(q_)), bs)
                bs.append(gen_ret(q_))
                run_a(gen_fm(q_, [6, 7]), bs)
                drain(bs)
                bs.append(gen_att(q_))
                run_a(gen_fm(q_, [8, 9, 10, 11]), bs)
                drain(bs)
            elif "e4" in taps:
                run_a(chain(gen_fm(q_, [0, 1]), gen_tm(q_), gen_fm(q_, [6, 7])), bs)
                bs.append(gen_ret(q_))
                bs.append(gen_att(q_))
                run_a(gen_fm(q_, [8, 9, 10, 11]), bs)
                drain(bs)
            elif "e0" not in taps:
                kb.set_pools([0, 1, 4, 5], [2, 3, 6, 7])
                run_a(gen_fm(q_, [6, 7]), bs)
                bs.append(gen_att(q_))
                run_a(chain(gen_fm(q_, [0, 1]), gen_tm(q_)), bs)
                drain(bs)
                kb.set_pools([0, 1], [2, 3], [4, 5, 6, 7])
                bs.append(gen_ret(q_))
                run_a(gen_fm(q_, [8, 9, 10, 11]), bs)
                drain(bs)
                kb.set_pools([0, 1, 4, 5, 6, 7], [2, 3])
            else:
                run_a(chain(gen_fm(q_, [0, 1]), gen_tm(q_)), bs)
                bs.append(gen_ret(q_))
                if "seq" in taps:
                    drain(bs)
                run_a(gen_fm(q_, [6, 7]), bs)
                bs.append(gen_att(q_))
                if "seq" in taps:
                    drain(bs)
                run_a(gen_fm(q_, [8, 9, 10, 11]), bs)
                drain(bs)
            if gi + 1 < len(groups):
                bs.append(gen_norm(*groups[gi + 1]))
            run_a(chain(gen_up(q_), gen_out(s_, q_, None)), bs)
            drain(bs)
        drain_bg()
        if "dest" in taps:
            tapo["dest"] = nc.dram_tensor("tap_dest", [128, NT, 2], I32, kind="ExternalOutput").ap()
            op(sp, lambda: nc.sync.dma_start(out=tapo["dest"], in_=dest_all[:]), r=[bdest], q=qsp)
            tapo["wts"] = nc.dram_tensor("tap_wts", [128, NT, 2], F32, kind="ExternalOutput").ap()
            op(sp, lambda: nc.sync.dma_start(out=tapo["wts"], in_=wts[:]), r=[bwts], q=qsp)

        if stop_after is None:
            kb.barrier()
            es_mix.close()
            es_moe = ExitStack()
            kb.cur = es_moe
            NSL = CAP // 128
            wg = [sb(f"wg{i}", [128, 8, DE], BF16) for i in range(2)]
            wu = [sb(f"wu{i}", [128, 8, DE], BF16) for i in range(2)]
            wd = [sb(f"wd{i}", [128, 4, D], BF16) for i in range(2)]
            bwg, bwu, bwd = [Buf(), Buf()], [Buf(), Buf()], [Buf(), Buf()]
            Xe = [sb(f"Xe{i}", [128, NSL, D], BF16) for i in range(2)]
            bXe = [Buf(), Buf()]
            XT = [sb(f"XT{i}", [128, 8, CAP], BF16) for i in range(2)]
            bXT = [[Buf() for _ in range(NSL)] for _ in range(2)]
            tge = [sb(f"tge{i}", [128, CAP], F32) for i in range(2)]
            btge = [Buf(), Buf()]
            sgu = [sb(f"sgu{i}", [128, CAP], F32) for i in range(2)]
            bsgu = [Buf(), Buf()]
            actT = [sb(f"actT{i}", [128, 4, CAP], BF16) for i in range(2)]
            bactT = [[Buf() for _ in range(4)] for _ in range(2)]
            Yt = [sb(f"Yt{i}", [128, D], BF16) for i in range(2)]
            bYt = [Buf(), Buf()]
            yti = [0]

            def load_expert(e):
                i = e % 2
                op(pool, lambda: G.dma_start(out=wg[i][:], in_=w_ge[e].rearrange("(k p) f -> p k f", p=128)), w=[bwg[i]], q=qpl)
                op(pool, lambda: G.dma_start(out=wu[i][:], in_=w_ue[e].rearrange("(k p) f -> p k f", p=128)), w=[bwu[i]], q=qpl)
                op(pool, lambda: G.dma_start(out=wd[i][:], in_=w_de[e].rearrange("(k p) n -> p k n", p=128)), w=[bwd[i]], q=qpl)
                op(sp, lambda: nc.sync.dma_start(out=Xe[i][:], in_=Xg[e * CAP:(e + 1) * CAP, :].rearrange("(s p) d -> p s d", p=128)),
                   r=[bXg], w=[bXe[i]], q=qsp)

            def transpose_slots(e):
                i = e % 2
                for sl in range(NSL):
                    bt, bb = kb.bank()
                    btv = bt[:].bitcast(BF16).rearrange("p (k c) -> p k c", k=8)

                    def trx(btv=btv, sl=sl):
                        ins = None
                        for k in range(8):
                            ins = P_.transpose(out=btv[:, k, :], in_=Xe[i][:, sl, k * 128:(k + 1) * 128], identity=identb[:])
                        return ins
                    op(pe, trx, r=[bXe[i], bidb], w=[bb])
                    if sl % 2 == 0:
                        op(act, lambda btv=btv, sl=sl: S.copy(out=XT[i][:, :, sl * 128:(sl + 1) * 128], in_=btv), r=[bb], w=[bXT[i][sl]])
                    else:
                        op(dve, lambda btv=btv, sl=sl: V.tensor_copy(out=XT[i][:, :, sl * 128:(sl + 1) * 128], in_=btv), r=[bb], w=[bXT[i][sl]])

            load_expert(0)
            transpose_slots(0)
            for e in range(NE):
                i = e % 2
                if e + 1 < NE:
                    load_expert(e + 1)
                for f in range(4):
                    btg_, bbg = kb.bank()
                    btu, bbu = kb.bank()

                    def mmg(btg_=btg_, f=f):
                        ins = None
                        for k in range(8):
                            ins = P_.matmul(btg_[:, 0:CAP], lhsT=wg[i][:, k, f * 128:(f + 1) * 128], rhs=XT[i][:, k, :], start=(k == 0), stop=(k == 7))
                        return ins

                    def mmu(btu=btu, f=f):
                        ins = None
                        for k in range(8):
                            ins = P_.matmul(btu[:, 0:CAP], lhsT=wu[i][:, k, f * 128:(f + 1) * 128], rhs=XT[i][:, k, :], start=(k == 0), stop=(k == 7))
                        return ins
                    op(pe, mmg, r=[bwg[i]] + bXT[i], w=[bbg])
                    op(pe, mmu, r=[bwu[i]] + bXT[i], w=[bbu])
                    fi = f % 2
                    op(act, lambda btg_=btg_, fi=fi: S.activation(out=tge[fi][:], in_=btg_[:, 0:CAP], func=AF.Tanh, scale=0.5), r=[bbg], w=[btge[fi]])
                    op(dve, lambda btg_=btg_, fi=fi: V.scalar_tensor_tensor(out=sgu[fi][:], in0=tge[fi][:], scalar=1.0, in1=btg_[:, 0:CAP], op0=ALU.add, op1=ALU.mult),
                       r=[bbg, btge[fi]], w=[bsgu[fi]])
                    op(dve, lambda btu=btu, fi=fi, f=f: V.tensor_tensor(out=actT[i][:, f, :], in0=sgu[fi][:], in1=btu[:, 0:CAP], op=ALU.mult),
                       r=[bbu, bsgu[fi]], w=[bactT[i][f]])
                if e + 1 < NE:
                    transpose_slots(e + 1)
                for sl in range(NSL):
                    yi = yti[0] % 2
                    yti[0] += 1
                    for hf in range(2):
                        bt, bb = kb.bank()

                        def mmd(bt=bt, sl=sl, hf=hf):
                            ins = None
                            for f in range(4):
                                ins = P_.matmul(bt[:], lhsT=actT[i][:, f, sl * 128:(sl + 1) * 128], rhs=wd[i][:, f, hf * 512:(hf + 1) * 512], start=(f == 0), stop=(f == 3))
                            return ins
                        op(pe, mmd, r=bactT[i] + [bwd[i]], w=[bb])
                        if hf == 0:
                            op(act, lambda bt=bt, yi=yi: S.copy(out=Yt[yi][:, 0:512], in_=bt[:]), r=[bb], pw=[bYt[yi]])
                        else:
                            op(dve, lambda bt=bt, yi=yi: V.tensor_copy(out=Yt[yi][:, 512:1024], in_=bt[:]), r=[bb], pw=[bYt[yi]])
                    r0 = e * CAP + sl * 128
                    op(sp, lambda yi=yi, r0=r0: nc.sync.dma_start(out=Yg[r0:r0 + 128, :], in_=Yt[yi][:]), r=[bYt[yi]], pw=[bYg], q=qsp)

            kb.barrier()
            es_moe.close()
            es_fin = ExitStack()
            kb.cur = es_fin
            NB_ = 4
            y1 = [sb(f"y1_{i}", [128, D], BF16) for i in range(NB_)]
            y2 = [sb(f"y2_{i}", [128, D], BF16) for i in range(NB_)]
            xr = [sb(f"xr{i}", [128, D], F32) for i in range(NB_)]
            by1, by2, bxr = [Buf() for _ in range(NB_)], [Buf() for _ in range(NB_)], [Buf() for _ in range(NB_)]
            ot = [sb(f"ot{i}", [128, D], F32) for i in range(2)]
            bot = [Buf(), Buf()]
            y1s = [sb(f"y1s{i}", [128, D], F32) for i in range(2)]
            by1s = [Buf(), Buf()]
            jk = sb("jk", [128, D], BF16)
            bjk = Buf()
            fs = sb("fs", [128, NT, 2], F32)
            bfs = [Buf() for _ in range(NT)]

            def fin_load(tt):
                i = tt % NB_
                op(pool, lambda: G.indirect_dma_start(out=y1[i][:], out_offset=None, in_=Yg[:, :],
                                                      in_offset=bass.IndirectOffsetOnAxis(ap=dest_all[:, tt, 0:1], axis=0),
                                                      bounds_check=breg2, oob_is_err=False), r=[bYg, bdest], w=[by1[i]], q=qpl)
                op(pool, lambda: G.indirect_dma_start(out=y2[i][:], out_offset=None, in_=Yg[:, :],
                                                      in_offset=bass.IndirectOffsetOnAxis(ap=dest_all[:, tt, 1:2], axis=0),
                                                      bounds_check=breg2, oob_is_err=False), r=[bYg, bdest], w=[by2[i]], q=qpl)
                op(sp, lambda: nc.sync.dma_start(out=xr[i][:], in_=X2[tt * 128:(tt + 1) * 128, :]), r=[bX2], w=[bxr[i]], q=qsp)

            def fin_a(tt):
                i = tt % NB_
                k_ = tt % 2
                op(act, lambda: S.activation(out=y1s[k_][:], in_=y1[i][:], func=AF.Copy, scale=wts[:, tt, 0:1]), r=[bwts, by1[i]], w=[by1s[k_]])
                op(dve, lambda: V.scalar_tensor_tensor(out=xr[i][:], in0=y2[i][:], scalar=wts[:, tt, 1:2], in1=xr[i][:], op0=ALU.mult, op1=ALU.add),
                   r=[by2[i], bwts], w=[bxr[i]])
                op(dve, lambda: V.tensor_tensor(out=xr[i][:], in0=xr[i][:], in1=y1s[k_][:], op=ALU.add), r=[by1s[k_]], w=[bxr[i]])
                op(act, lambda: S.activation(out=jk[:], in_=xr[i][:], func=AF.Square, accum_out=fs[:, tt, 0:1]), r=[bxr[i]], w=[bjk, bfs[tt]])
                op(dve, lambda: V.tensor_scalar(out=fs[:, tt, 0:1], in0=fs[:, tt, 0:1], scalar1=1.0 / D, scalar2=EPS, op0=ALU.mult, op1=ALU.add), w=[bfs[tt]])
                op(pool, lambda: G.tensor_tensor(out=fs[:, tt, 1:2], in0=fs[:, tt, 0:1], in1=mhalf[:, 0:1], op=ALU.pow), r=[bones], w=[bfs[tt]])

            def fin_b(tt):
                i = tt % NB_
                o = tt % 2
                op(act, lambda: S.activation(out=ot[o][:], in_=xr[i][:], func=AF.Copy, scale=fs[:, tt, 1:2]), r=[bxr[i], bfs[tt]], w=[bot[o]])
                op(dve, lambda: V.tensor_tensor(out=ot[o][:], in0=ot[o][:], in1=gfin[:], op=ALU.mult), r=[bgfin], w=[bot[o]])
                op(sp, lambda: nc.sync.dma_start(out=out[tt * 128:(tt + 1) * 128, :], in_=ot[o][:]), r=[bot[o]], q=qsp)

            for tt in range(min(3, NT)):
                fin_load(tt)
            for tt in range(NT):
                fin_a(tt)
                if tt >= 1:
                    fin_b(tt - 1)
                if tt + 3 < NT:
                    fin_load(tt + 3)
            fin_b(NT - 1)

        for qq in (kb.qsp, kb.qpl, kb.qact, kb.qcast):
            for sem, c in zip(qq.sems, qq.cnt):
                if c:
                    sp.wait(sem, 16 * c)
        for E in (pe, act, dve, pool):
            if E.sem is not None:
                sp.wait(E.sem, E.cnt)
        if kb.cur is not es:
            kb.cur.close()
    return nc


def kernel(**inputs):
    n = N_CORES
    nseq = inputs["x"].shape[0] // n
    nc = build_nc(nseq=nseq)
    consts = host_consts()
    shared = {}
    for k in ("norm_mix_g", "w_in", "b_in", "attn_sinks", "w_attn_up", "w_ret_up", "w_out", "norm_ffn_g", "w_group_router",
              "b_group_router", "w_expert_router", "b_expert_router", "w_gate_e", "w_up_e", "w_down_e"):
        a = np.asarray(inputs[k])[0]
        shared[k] = np.ascontiguousarray(a.reshape(1, -1) if a.ndim == 1 else a, dtype=np.float32)
    shared["norm_final_g"] = np.ascontiguousarray(np.asarray(inputs["norm_final_g"]).reshape(1, -1), dtype=np.float32)
    shared.update(consts)
    x = np.asarray(inputs["x"], dtype=np.float32)
    in_maps = []
    for c in range(n):
        m = dict(shared)
        m["x"] = np.ascontiguousarray(x[c * nseq:(c + 1) * nseq].reshape(nseq * SEQ, D))
        in_maps.append(m)
    res = run_bass_kernel_spmd(nc, in_maps, core_ids=list(range(n)))
    outs = [np.asarray(r["out"]).reshape(nseq, SEQ, D) for r in res.results]
    return np.concatenate(outs, axis=0).astype(np.float32)
```
